# Optimizing a Trainium2 kernel written in Bass

```python
import math
import jax
import jax.numpy as jnp
from jax import lax
import numpy as np

D_MODEL = 1024
BATCH = 8
SEQ = 4096
DEPTH = 1

RET_HEADS = 4
RET_HEAD_DIM = 256
RET_WIDTH = RET_HEADS * RET_HEAD_DIM
RET_CHUNK = 128
ROPE_BASE = 10000.0
ATT_HEADS = 16
ATT_HEAD_DIM = 64
ATT_WIDTH = ATT_HEADS * ATT_HEAD_DIM
DILATION_PATTERNS = ((128, 1), (512, 4), (2048, 16))
REL_BUCKETS = 32
REL_MAX_DISTANCE = 1024
N_EXPERTS = 256
TOP_K = 8
N_GROUPS = 8
TOPK_GROUPS = 4
EXPERT_HIDDEN = 256
SHARED_HIDDEN = 256
ROUTED_SCALE = 2.5
MOE_BLOCK = 128
NORM_EPS = 1e-6
PROJ_WIDTH = 4 * RET_WIDTH + 3 * ATT_WIDTH + 2 * D_MODEL

kernel_name = 'hybrid_retention_dilated_attn_moe_block'


def rms_norm(x, gain):
    xf = x.astype(jnp.float32)
    y = xf * lax.rsqrt(jnp.mean(xf * xf, axis=-1, keepdims=True) + NORM_EPS)
    return (y * gain.astype(jnp.float32)).astype(x.dtype)


def modulate(h, shift, scale):
    return h * (1.0 + scale[:, None, :]) + shift[:, None, :]


def swiglu(h, w_gate, w_up, w_down):
    return (jax.nn.silu(h @ w_gate) * (h @ w_up)) @ w_down


def rotary(t):
    seq, dh = t.shape[1], t.shape[-1]
    half = dh // 2
    inv_freq = ROPE_BASE ** (-jnp.arange(half, dtype=jnp.float32) / half)
    ang = jnp.arange(seq, dtype=jnp.float32)[:, None] * inv_freq[None, :]
    cos = jnp.cos(ang)[None, :, None, :].astype(t.dtype)
    sin = jnp.sin(ang)[None, :, None, :].astype(t.dtype)
    t1, t2 = t[..., :half], t[..., half:]
    return jnp.concatenate([t1 * cos - t2 * sin, t1 * sin + t2 * cos], axis=-1)


def retention_scan(q, k, v, log_gamma, strict):
    bsz, heads, seq, dk = q.shape
    dv = v.shape[-1]
    n_chunks = seq // RET_CHUNK
    dt = q.dtype
    pos = jnp.arange(RET_CHUNK, dtype=jnp.float32)
    diff = pos[:, None] - pos[None, :]
    allowed = (diff > 0) if strict else (diff >= 0)
    lg = log_gamma[:, None, None]
    intra = jnp.where(allowed[None], jnp.exp(lg * jnp.where(allowed, diff, 0.0)[None]), 0.0).astype(dt)
    q_scale = jnp.exp(log_gamma[:, None] * (pos[None, :] + 1.0)).astype(dt)[None, :, :, None]
    k_scale = jnp.exp(log_gamma[:, None] * (RET_CHUNK - 1.0 - pos[None, :])).astype(dt)[None, :, :, None]
    chunk_decay = jnp.exp(log_gamma * RET_CHUNK).astype(dt)[None, :, None, None]

    def chunks(t):
        return t.reshape(bsz, heads, n_chunks, RET_CHUNK, t.shape[-1]).transpose(2, 0, 1, 3, 4)

    def step(state, inp):
        qn, kn, vn = inp
        scores = jnp.einsum('bhid,bhjd->bhij', qn, kn) * intra[None]
        out = (jnp.einsum('bhij,bhje->bhie', scores, vn)
               + jnp.einsum('bhid,bhde->bhie', qn * q_scale, state))
        state = state * chunk_decay + jnp.einsum('bhjd,bhje->bhde', kn * k_scale, vn)
        return state, out

    state0 = jnp.zeros((bsz, heads, dk, dv), dt)
    _, out = lax.scan(step, state0, (chunks(q), chunks(k), chunks(v)))
    return out.transpose(1, 2, 0, 3, 4).reshape(bsz, heads, seq, dv)


def retention_mixer(q, k, v, g, ret_decay):
    bsz, seq, _ = q.shape

    def heads(t):
        return t.reshape(bsz, seq, RET_HEADS, RET_HEAD_DIM)

    q = rotary(heads(q)).transpose(0, 2, 1, 3)
    k = (rotary(heads(k)) * (RET_HEAD_DIM ** -0.5)).transpose(0, 2, 1, 3)
    v = heads(v).transpose(0, 2, 1, 3)
    log_gamma = jnp.log1p(-jnp.exp(ret_decay.astype(jnp.float32)))
    fwd = retention_scan(q, k, v, log_gamma[0], strict=False)
    bwd = jnp.flip(retention_scan(jnp.flip(q, 2), jnp.flip(k, 2), jnp.flip(v, 2), log_gamma[1], strict=True), 2)
    o = (fwd + bwd).astype(jnp.float32)
    mu = jnp.mean(o, axis=-1, keepdims=True)
    var = jnp.mean(jnp.square(o - mu), axis=-1, keepdims=True)
    o = ((o - mu) * lax.rsqrt(var + NORM_EPS)).astype(g.dtype)
    o = o.transpose(0, 2, 1, 3).reshape(bsz, seq, RET_WIDTH)
    return o * jax.nn.silu(g)


def t5_bucket(rel):
    half = REL_BUCKETS // 2
    max_exact = half // 2
    n = jnp.abs(rel)
    large = max_exact + (jnp.log(jnp.maximum(n, 1).astype(jnp.float32) / max_exact)
                         / math.log(REL_MAX_DISTANCE / max_exact) * (half - max_exact)).astype(jnp.int32)
    large = jnp.minimum(large, half - 1)
    return jnp.where(rel > 0, half, 0) + jnp.where(n < max_exact, n, large)


def dilated_band_attention(q, k, v, t5_bias, window, dilation):
    bsz, seq, heads, dh = q.shape
    radius = window // (2 * dilation)
    blk = radius
    length = seq // dilation
    n_blk = -(-length // blk)
    padded = n_blk * blk

    def to_strided(t):
        t = t.reshape(bsz, length, dilation, heads, dh).transpose(0, 2, 3, 1, 4)
        return jnp.pad(t, ((0, 0), (0, 0), (0, 0), (0, padded - length), (0, 0)))

    def band(t):
        t = jnp.pad(to_strided(t), ((0, 0), (0, 0), (0, 0), (blk, blk), (0, 0)))
        t = t.reshape(bsz, dilation, heads, n_blk + 2, blk, dh)
        return jnp.concatenate([t[:, :, :, :-2], t[:, :, :, 1:-1], t[:, :, :, 2:]], axis=4)

    qb = to_strided(q).reshape(bsz, dilation, heads, n_blk, blk, dh)
    kb, vb = band(k), band(v)
    qi = jnp.arange(blk, dtype=jnp.int32)
    kj = jnp.arange(3 * blk, dtype=jnp.int32)
    rel = kj[None, :] - blk - qi[:, None]
    key_idx = jnp.arange(n_blk, dtype=jnp.int32)[:, None] * blk - blk + kj[None, :]
    valid = (jnp.abs(rel) <= radius)[None] & ((key_idx >= 0) & (key_idx < length))[:, None, :]
    bias = t5_bias[t5_bucket(rel * dilation)].astype(jnp.float32).transpose(2, 0, 1)
    s = jnp.einsum('brhnqe,brhnke->brhnqk', qb, kb).astype(jnp.float32) * (dh ** -0.5)
    s = s + bias[None, None, :, None]
    s = jnp.where(valid[None, None, None], s, -jnp.inf)
    lse = jax.nn.logsumexp(s, axis=-1)
    p = jnp.exp(s - lse[..., None]).astype(v.dtype)
    o = jnp.einsum('brhnqk,brhnke->brhnqe', p, vb)
    o = o.reshape(bsz, dilation, heads, padded, dh)[:, :, :, :length]
    o = o.transpose(0, 3, 1, 2, 4).reshape(bsz, seq, heads, dh)
    lse = lse.reshape(bsz, dilation, heads, padded)[..., :length].transpose(0, 3, 1, 2).reshape(bsz, seq, heads)
    return o, lse


def dilated_attention(q, k, v, t5_bias):
    bsz, seq, _ = q.shape

    def heads(t):
        return t.reshape(bsz, seq, ATT_HEADS, ATT_HEAD_DIM)

    q, k, v = heads(q), heads(k), heads(v)
    outs, lses = [], []
    for window, dilation in DILATION_PATTERNS:
        o, lse = dilated_band_attention(q, k, v, t5_bias, window, dilation)
        outs.append(o)
        lses.append(lse)
    weights = jax.nn.softmax(jnp.stack(lses, axis=-1), axis=-1).astype(q.dtype)
    o = jnp.sum(jnp.stack(outs, axis=-2) * weights[..., None], axis=-2)
    return o.reshape(bsz, seq, ATT_WIDTH)


def token_mixer(h, w_in, ret_decay, t5_bias, w_ret_up, w_att_up, w_o):
    widths = [RET_WIDTH] * 4 + [ATT_WIDTH] * 3 + [D_MODEL] * 2
    splits = np.cumsum(widths)[:-1].tolist()
    proj = h @ w_in
    rq, rk, rv, rg, aq, ak, av, gate_r, gate_a = jnp.split(proj, splits, axis=-1)
    y_ret = retention_mixer(rq, rk, rv, rg, ret_decay) @ w_ret_up
    y_att = dilated_attention(aq, ak, av, t5_bias) @ w_att_up
    merged = jax.nn.sigmoid(gate_r) * y_ret + jax.nn.sigmoid(gate_a) * y_att
    return merged @ w_o


def route(h, w_router, router_bias):
    n_tok = h.shape[0]
    per_group = N_EXPERTS // N_GROUPS
    scores = jax.nn.sigmoid((h @ w_router).astype(jnp.float32))
    choice = scores + router_bias.astype(jnp.float32)
    group_score = jnp.sum(lax.top_k(choice.reshape(n_tok, N_GROUPS, per_group), 2)[0], axis=-1)
    _, top_groups = lax.top_k(group_score, TOPK_GROUPS)
    group_mask = jnp.sum(jax.nn.one_hot(top_groups, N_GROUPS, dtype=jnp.float32), axis=-2) > 0
    expert_mask = jnp.repeat(group_mask, per_group, axis=-1)
    _, expert_idx = lax.top_k(jnp.where(expert_mask, choice, -jnp.inf), TOP_K)
    gate = jnp.take_along_axis(scores, expert_idx, axis=-1)
    gate = gate / jnp.sum(gate, axis=-1, keepdims=True) * ROUTED_SCALE
    return expert_idx.astype(jnp.int32), gate


def routed_experts(h, expert_idx, gate, w_gate, w_up, w_down):
    n_tok, d = h.shape
    n_assign = n_tok * TOP_K
    n_blocks = -(-n_assign // MOE_BLOCK) + N_EXPERTS
    buf = n_blocks * MOE_BLOCK
    flat_e = expert_idx.reshape(-1)
    flat_t = jnp.repeat(jnp.arange(n_tok, dtype=jnp.int32), TOP_K)
    flat_w = gate.reshape(-1).astype(h.dtype)
    order = jnp.argsort(flat_e)
    e_s, t_s, w_s = flat_e[order], flat_t[order], flat_w[order]
    counts = jnp.bincount(flat_e, length=N_EXPERTS).astype(jnp.int32)
    padded = (counts + MOE_BLOCK - 1) // MOE_BLOCK * MOE_BLOCK
    pad_end = jnp.cumsum(padded)
    pad_start = pad_end - padded
    start = jnp.cumsum(counts) - counts
    dest = pad_start[e_s] + jnp.arange(n_assign, dtype=jnp.int32) - start[e_s]
    tok_buf = jnp.full((buf,), n_tok, jnp.int32).at[dest].set(t_s)
    w_buf = jnp.zeros((buf,), h.dtype).at[dest].set(w_s)
    block_expert = jnp.clip(jnp.searchsorted(pad_end, jnp.arange(n_blocks, dtype=jnp.int32) * MOE_BLOCK, side='right'),
                            0, N_EXPERTS - 1)
    h_pad = jnp.concatenate([h, jnp.zeros((1, d), h.dtype)], axis=0)

    def block(args):
        tok, wt, e = args
        xb = h_pad[tok]
        hid = jax.nn.silu(xb @ w_gate[e]) * (xb @ w_up[e])
        return (hid @ w_down[e]) * wt[:, None]

    y = lax.map(block, (tok_buf.reshape(n_blocks, MOE_BLOCK), w_buf.reshape(n_blocks, MOE_BLOCK), block_expert))
    return jax.ops.segment_sum(y.reshape(buf, d), tok_buf, num_segments=n_tok + 1)[:n_tok]


def setup_inputs(seed: int = 0) -> dict:
    key = jax.random.key(seed)
    ks = jax.random.split(key, 24)
    f32 = jnp.float32

    def nrm(k, shape, scale):
        return jax.random.normal(k, shape, f32) * scale

    decay_init = jnp.asarray(np.log(2.0 ** (-5.0 - np.arange(RET_HEADS))), f32)
    return {
        'x': nrm(ks[0], (BATCH, SEQ, D_MODEL), 1.0),
        'c': nrm(ks[1], (BATCH, D_MODEL), 1.0),
        'w_ada': nrm(ks[2], (DEPTH, D_MODEL, 6 * D_MODEL), 0.5 * D_MODEL ** -0.5),
        'b_ada': nrm(ks[3], (DEPTH, 6 * D_MODEL), 0.02),
        'norm_mix': 1.0 + nrm(ks[4], (DEPTH, D_MODEL), 0.02),
        'w_in': nrm(ks[5], (DEPTH, D_MODEL, PROJ_WIDTH), D_MODEL ** -0.5),
        'ret_decay': decay_init[None, None, :] + nrm(ks[6], (DEPTH, 2, RET_HEADS), 0.1),
        't5_bias': nrm(ks[7], (REL_BUCKETS, ATT_HEADS), 0.5),
        'w_ret_up': nrm(ks[8], (DEPTH, RET_WIDTH, D_MODEL), RET_WIDTH ** -0.5),
        'w_att_up': nrm(ks[9], (DEPTH, ATT_WIDTH, D_MODEL), ATT_WIDTH ** -0.5),
        'w_o': nrm(ks[10], (DEPTH, D_MODEL, D_MODEL), D_MODEL ** -0.5),
        'norm_ffn': 1.0 + nrm(ks[11], (DEPTH, D_MODEL), 0.02),
        'w_router': nrm(ks[12], (DEPTH, D_MODEL, N_EXPERTS), D_MODEL ** -0.5),
        'router_bias': nrm(ks[13], (DEPTH, N_EXPERTS), 0.01),
        'w_gate': nrm(ks[14], (DEPTH, N_EXPERTS, D_MODEL, EXPERT_HIDDEN), D_MODEL ** -0.5),
        'w_up': nrm(ks[15], (DEPTH, N_EXPERTS, D_MODEL, EXPERT_HIDDEN), D_MODEL ** -0.5),
        'w_down': nrm(ks[16], (DEPTH, N_EXPERTS, EXPERT_HIDDEN, D_MODEL), EXPERT_HIDDEN ** -0.5),
        'ws_gate': nrm(ks[17], (DEPTH, D_MODEL, SHARED_HIDDEN), D_MODEL ** -0.5),
        'ws_up': nrm(ks[18], (DEPTH, D_MODEL, SHARED_HIDDEN), D_MODEL ** -0.5),
        'ws_down': nrm(ks[19], (DEPTH, SHARED_HIDDEN, D_MODEL), SHARED_HIDDEN ** -0.5),
        'norm_final': 1.0 + nrm(ks[20], (D_MODEL,), 0.02),
    }


def reference(x, c, w_ada, b_ada, norm_mix, w_in, ret_decay, t5_bias, w_ret_up, w_att_up, w_o,
              norm_ffn, w_router, router_bias, w_gate, w_up, w_down, ws_gate, ws_up, ws_down, norm_final):
    bsz, seq, d = x.shape
    cond = jax.nn.silu(c)
    for layer in range(DEPTH):
        mod = cond @ w_ada[layer] + b_ada[layer]
        shift_m, scale_m, gate_m, shift_f, scale_f, gate_f = jnp.split(mod, 6, axis=-1)
        h = modulate(rms_norm(x, norm_mix[layer]), shift_m, scale_m)
        x = x + gate_m[:, None, :] * token_mixer(h, w_in[layer], ret_decay[layer], t5_bias,
                                                 w_ret_up[layer], w_att_up[layer], w_o[layer])
        h = modulate(rms_norm(x, norm_ffn[layer]), shift_f, scale_f).reshape(bsz * seq, d)
        expert_idx, gate = route(h, w_router[layer], router_bias[layer])
        y = (routed_experts(h, expert_idx, gate, w_gate[layer], w_up[layer], w_down[layer])
             + swiglu(h, ws_gate[layer], ws_up[layer], ws_down[layer]))
        x = x + gate_f[:, None, :] * y.reshape(bsz, seq, d)
    return rms_norm(x, norm_final)
```

```python
import math
from contextlib import ExitStack
import numpy as np
import ml_dtypes
import concourse.bass as bass
import concourse.mybir as mybir
from concourse.bass_utils import run_bass_kernel_spmd

F32 = mybir.dt.float32
BF16 = mybir.dt.bfloat16
I32 = mybir.dt.int32
U32 = mybir.dt.uint32
AF = mybir.ActivationFunctionType
ALU = mybir.AluOpType
AX = mybir.AxisListType

D = 1024
S = 4096
NT = 32
PROJ = 9216
NE = 256
BS = 256
TPB = BS // 128
NBLK = 32768 // BS + 256
NSLOT = NBLK * BS
EPS = 1e-6


class Tok:
    __slots__ = ("w", "r", "name")

    def __init__(self, name=""):
        self.w = None
        self.r = []
        self.name = name


class Op:
    __slots__ = ("eng", "fn", "deps", "dma", "sig", "sem", "val", "idx", "prewait", "ph")

    def __init__(self, eng, fn, dma):
        self.eng = eng
        self.fn = fn
        self.dma = dma
        self.deps = []
        self.sig = False
        self.sem = None
        self.val = 0
        self.prewait = None


ENGS = ("pe", "act", "dve", "pool", "sp")


class Sched:
    def __init__(self, nc, stack):
        self.nc = nc
        self.stack = stack
        self.ops = []
        self.eobj = {"pe": nc.tensor, "act": nc.scalar, "dve": nc.vector,
                     "pool": nc.gpsimd, "sp": nc.sync}
        self.dma_sems = {}
        for q, n in (("sp", 24), ("act", 8), ("pool", 12)):
            self.dma_sems[q] = [[stack.enter_context(nc.semaphore(f"d{q}{i}")), 0, None]
                                for i in range(n)]
        self.dma_rr = {"sp": 0, "act": 0, "pool": 0}
        self.phase_i = 0

    def op(self, eng, fn, reads=(), writes=(), dma=False):
        o = Op(eng, fn, dma)
        o.idx = len(self.ops)
        o.ph = self.phase_i
        deps = set()

        def same(d):
            return (not dma) and (not d.dma) and d.eng == eng

        for t in reads:
            if t.w is not None:
                deps.add(t.w)
        for t in writes:
            if t.w is not None and not same(t.w):
                deps.add(t.w)
            for r in t.r:
                if not same(r):
                    deps.add(r)
        deps.discard(o)
        o.deps = [d for d in deps if d.ph == self.phase_i]
        for t in reads:
            t.r.append(o)
        for t in writes:
            t.w = o
            t.r = []
        self.ops.append(o)
        return o

    def flush(self, name):
        nc = self.nc
        ops = self.ops
        self.ops = []
        if not ops:
            return
        for o in ops:
            for d in o.deps:
                if not d.dma:
                    if d.eng == "pe" and o.eng == "pe" and not o.dma:
                        continue
                    d.sig = True
        with ExitStack() as st:
            esem = {e: st.enter_context(nc.semaphore(f"p{self.phase_i}{e}")) for e in ENGS}
            ecount = {e: 0 for e in ENGS}
            for o in ops:
                if o.dma:
                    pool = self.dma_sems[o.eng]
                    i = self.dma_rr[o.eng]
                    self.dma_rr[o.eng] = (i + 1) % len(pool)
                    ent = pool[i]
                    o.prewait = (ent[0], ent[1]) if ent[1] > 0 else None
                    ent[1] += 16
                    o.sem = ent[0]
                    o.val = ent[1]
                elif o.sig:
                    ecount[o.eng] += 1
                    o.sem = esem[o.eng]
                    o.val = ecount[o.eng]
            per = {e: [o for o in ops if o.eng == e] for e in ENGS}
            with nc.Block() as block:
                def run(e, eng):
                    known = {}

                    def wait(sem, val):
                        k = id(sem)
                        if known.get(k, 0) >= val:
                            return
                        known[k] = val
                        eng.wait_ge(sem, val)

                    for o in per[e]:
                        for d in o.deps:
                            if d.dma:
                                wait(d.sem, d.val)
                            else:
                                if d.eng == "pe" and e == "pe" and not o.dma:
                                    continue
                                wait(d.sem, d.val)
                        if o.dma and o.prewait is not None:
                            wait(*o.prewait)
                        inst = o.fn()
                        if o.dma:
                            inst.then_inc(o.sem, 16)
                        elif o.sig:
                            inst.then_inc(o.sem, 1)
                    if e in self.dma_sems:
                        for ent in self.dma_sems[e]:
                            if ent[1] > 0:
                                wait(ent[0], ent[1])

                if per["pe"]:
                    @block.tensor
                    def _(eng):
                        run("pe", eng)
                if per["act"]:
                    @block.scalar
                    def _(eng):
                        run("act", eng)
                if per["dve"]:
                    @block.vector
                    def _(eng):
                        run("dve", eng)
                if per["pool"]:
                    @block.gpsimd
                    def _(eng):
                        run("pool", eng)
                if per["sp"]:
                    @block.sync
                    def _(eng):
                        run("sp", eng)
        self.phase_i += 1


def _bf16(a):
    return np.ascontiguousarray(a).astype(ml_dtypes.bfloat16)


class K:
    pass


def build_nc(stop_after="H", debug=()):
    nc = bass.Bass("TRN2", target_bir_lowering=False)
    k = K()
    k.nc = nc
    k.debug = set(debug)

    def din(name, shape, dt=F32):
        return nc.dram_tensor(name, list(shape), dt, kind="ExternalInput").ap()

    def dscr(name, shape, dt):
        kind = "ExternalOutput" if name in k.debug else "Internal"
        return nc.dram_tensor(name, list(shape), dt, kind=kind).ap()

    k.xT = din("xT", [8, 128, S])
    k.c_l = din("c_l", [128, 8])
    k.w_ada = din("w_ada", [D, 6 * D])
    k.b_ada_l = din("b_ada_l", [128, 48])
    k.b_gf_bc = din("b_gf_bc", [128, D])
    k.nmix_l = din("nmix_l", [128, 8])
    k.nffn_l = din("nffn_l", [128, 8])
    k.w_in = din("w_in", [D, PROJ])
    k.rdecay_bc = din("rdecay_bc", [128, 8])
    k.t5b = din("t5b", [8, 128, 6 * 384])
    k.w_ret_up = din("w_ret_up", [D, D])
    k.w_att_up = din("w_att_up", [D, D])
    k.w_o = din("w_o", [D, D])
    k.w_router = din("w_router", [D, NE])
    k.rbias_bc = din("rbias_bc", [128, NE])
    if stop_after >= "G":
        k.w_all = din("w_all", [NE * 128, 6144])
    k.ws_gate = din("ws_gate", [D, 256])
    k.ws_up = din("ws_up", [D, 256])
    k.ws_down = din("ws_down", [256, D])
    k.nfin_bc = din("nfin_bc", [128, D])
    k.cosT = din("cosT", [128, S])
    k.sinT = din("sinT", [128, S])
    k.cst_f32 = din("cst_f32", [128, 1280])
    k.cst_bf = din("cst_bf", [128, 1024], BF16)
    k.cst_g = din("cst_g", [128, 1024])
    k.out = nc.dram_tensor("out", [S, D], F32, kind="ExternalOutput").ap()

    for nm in ("QF", "QB", "KF", "KB", "SG", "AQ", "AK", "GR", "GA", "ORET", "OATT"):
        setattr(k, nm, dscr(nm, [8, 128, S], BF16))
    k.RV = dscr("RV", [S, D], BF16)
    k.AV = dscr("AV", [S, D], BF16)
    k.XS = dscr("XS", [S, D], F32)
    if stop_after >= "F":
        k.XG = dscr("XG", [NSLOT, D], BF16)
        k.YE = dscr("YE", [NSLOT, D], BF16)
        k.SELB = dscr("SELB", [NT, 128, NE], BF16)
        k.H2TOK = dscr("H2TOK", [S, D], BF16)
    k.HT = dscr("HT", [8, 128, S], BF16) if "HT" in k.debug else None
    k.MOD = dscr("MOD", [128, 64], F32) if "MOD" in k.debug else None
    if "SLOT" in k.debug:
        k.SLOT = dscr("SLOT", [128, NT, 8], I32)
        k.GATE = dscr("GATE", [128, NT, 8], F32)
    if "IDX" in k.debug:
        k.IDX = dscr("IDX", [128, NBLK], I32)

    with ExitStack() as top:
        sch = Sched(nc, top)
        k.sch = sch
        k.top = top

        def sb(st, name, shape, dt):
            return st.enter_context(nc.sbuf_tensor(name, list(shape), dt))

        def ps(st, name, shape, dt=F32):
            return st.enter_context(nc.psum_tensor(name, list(shape), dt))

        k.sb = sb
        k.ps = ps
        k.cf = sb(top, "cf", [128, 1280], F32)
        k.cb = sb(top, "cb", [128, 1024], BF16)
        k.modv = sb(top, "modv", [128, 64], F32)
        k.gfbc = sb(top, "gfbc", [128, D], F32)
        k.dec = sb(top, "dec", [128, 24], F32)
        k.slot_all = sb(top, "slot_all", [128, NT, 8], I32)
        k.idx_all = sb(top, "idx_all", [128, NBLK], I32)
        k.t_idx = Tok()
        k.gate_all = sb(top, "gate_all", [128, NT, 8], F32)
        k.t_cf, k.t_cb, k.t_modv, k.t_gfbc, k.t_dec = Tok(), Tok(), Tok(), Tok(), Tok()
        k.t_slot, k.t_gate = Tok(), Tok()
        k.ident_f = k.cf[:, 0:128]
        k.ones_f = k.cf[:, 128:256]
        k.posF = k.cf[:, 256:768]
        k.posB = k.cf[:, 768:1280]
        k.ident_b = k.cb[:, 0:128]
        k.maskF = k.cb[:, 128:256]
        k.maskB = k.cb[:, 256:384]
        k.Ustrict = k.cb[:, 384:512]
        k.attmask = k.cb[:, 512:896]
        k.ones_b = None

        if stop_after == "A":
            phase_A(k)
            return nc
        with ExitStack() as stBC:
            k.hT = sb(stBC, "hT", [128, 8, S], BF16)
            k.t_hT = [Tok() for _ in range(8)]
            with ExitStack() as stAB:
                phase_A(k, stAB, flush=False)
                phase_B(k)
            if stop_after != "B":
                phase_C(k)
        if stop_after in ("B", "C"):
            return nc
        if "skipD" not in k.debug:
            phase_D(k)
        if stop_after == "D":
            return nc
        if "skipE" not in k.debug:
            phase_E(k)
        if stop_after == "E":
            return nc
        with ExitStack() as stF:
            phase_F0(k, stF)
            phase_F1(k)
            phase_F2(k)
            phase_F3(k)
        if stop_after == "F":
            return nc
        phase_G(k)
        phase_H(k)
    return nc


def phase_A(k, st_outer=None, flush=True):
    nc, sch = k.nc, k.sch
    with ExitStack() as st_own:
        st = st_outer if st_outer is not None else st_own
        sb, ps = k.sb, k.ps
        cond = sb(st, "cond", [128, 8], F32)
        cl = sb(st, "cl", [128, 8], F32)
        condbc = sb(st, "condbc", [128, 8, 128], F32)
        bl = sb(st, "bl", [128, 48], F32)
        nm = sb(st, "nm", [128, 16], F32)
        rd = sb(st, "rd", [128, 8], F32)
        bgf = sb(st, "bgf", [128, D], F32)
        wa = [sb(st, f"wa{i}", [128, 8, 512], F32) for i in range(2)]
        pmod = ps(st, "pmod", [128, 512])
        pgf = [ps(st, f"pgf{i}", [128, 512]) for i in range(2)]
        t_cond, t_cl, t_cbc, t_bl, t_nm, t_rd, t_bgf = (Tok() for _ in range(7))
        t_wa = [Tok(), Tok()]
        t_pmod, t_pgf = Tok(), [Tok(), Tok()]

        sch.op("sp", lambda: nc.sync.dma_start(out=k.cf[:, :], in_=k.cst_f32[:, :]), writes=[k.t_cf], dma=True)
        sch.op("sp", lambda: nc.sync.dma_start(out=k.cb[:, :], in_=k.cst_bf[:, :]), writes=[k.t_cb], dma=True)
        sch.op("sp", lambda: nc.sync.dma_start(out=cl[:, :], in_=k.c_l[:, :]), writes=[t_cl], dma=True)
        sch.op("sp", lambda: nc.sync.dma_start(out=bl[:, :], in_=k.b_ada_l[:, :]), writes=[t_bl], dma=True)
        sch.op("sp", lambda: nc.sync.dma_start(out=nm[:, 0:8], in_=k.nmix_l[:, :]), writes=[t_nm], dma=True)
        sch.op("sp", lambda: nc.sync.dma_start(out=nm[:, 8:16], in_=k.nffn_l[:, :]), writes=[t_nm], dma=True)
        sch.op("sp", lambda: nc.sync.dma_start(out=rd[:, :], in_=k.rdecay_bc[:, :]), writes=[t_rd], dma=True)
        sch.op("sp", lambda: nc.sync.dma_start(out=bgf[:, :], in_=k.b_gf_bc[:, :]), writes=[t_bgf], dma=True)
        sch.op("act", lambda: nc.scalar.activation(out=cond[:, :], in_=cl[:, :], func=AF.Silu),
               reads=[t_cl], writes=[t_cond])
        for kk in range(8):
            sch.op("dve", lambda kk=kk: nc.vector.tensor_scalar(
                out=condbc[:, kk, :], in0=k.ones_f, scalar1=cond[:, kk:kk + 1], scalar2=None, op0=ALU.mult),
                reads=[t_cond, k.t_cf], writes=[t_cbc])
        wv = k.w_ada.rearrange("(kk p) n -> p kk n", p=128)
        for s2 in range(12):
            b = s2 % 2
            s, hs = s2 // 2, s2 % 2
            sch.op("sp", lambda s2=s2, b=b: nc.sync.dma_start(out=wa[b][:, :, :], in_=wv[:, :, s2 * 512:(s2 + 1) * 512]),
                   writes=[t_wa[b]], dma=True)
            for j4 in range(4):
                j = hs * 4 + j4
                for kk in range(8):
                    sch.op("pe", lambda s=s, b=b, j=j, j4=j4, kk=kk: nc.tensor.matmul(
                        pmod[:, s * 8 + j:s * 8 + j + 1], lhsT=wa[b][:, kk, j4 * 128:(j4 + 1) * 128],
                        rhs=cond[:, kk:kk + 1], start=(kk == 0), stop=(kk == 7)),
                        reads=[t_wa[b], t_cond], writes=[t_pmod])
            if s == 5:
                h = hs
                for kk in range(8):
                    sch.op("pe", lambda b=b, h=h, kk=kk: nc.tensor.matmul(
                        pgf[h][:, :], lhsT=condbc[:, kk, :], rhs=wa[b][:, kk, :],
                        start=(kk == 0), stop=(kk == 7)),
                        reads=[t_wa[b], t_cbc], writes=[t_pgf[h]])
        sch.op("dve", lambda: nc.vector.tensor_tensor(out=k.modv[:, 0:48], in0=pmod[:, 0:48], in1=bl[:, :], op=ALU.add),
               reads=[t_pmod, t_bl], writes=[k.t_modv])
        for h in range(2):
            sch.op("dve", lambda h=h: nc.vector.tensor_tensor(
                out=k.gfbc[:, h * 512:(h + 1) * 512], in0=pgf[h][:, :], in1=bgf[:, h * 512:(h + 1) * 512], op=ALU.add),
                reads=[t_pgf[h], t_bgf], writes=[k.t_gfbc])
        sch.op("dve", lambda: nc.vector.scalar_tensor_tensor(
            out=k.modv[:, 48:56], in0=k.modv[:, 8:16], scalar=1.0, in1=nm[:, 0:8], op0=ALU.add, op1=ALU.mult),
            reads=[k.t_modv, t_nm], writes=[k.t_modv])
        sch.op("dve", lambda: nc.vector.scalar_tensor_tensor(
            out=k.modv[:, 56:64], in0=k.modv[:, 32:40], scalar=1.0, in1=nm[:, 8:16], op0=ALU.add, op1=ALU.mult),
            reads=[k.t_modv, t_nm], writes=[k.t_modv])
        sch.op("act", lambda: nc.scalar.activation(out=rd[:, :], in_=rd[:, :], func=AF.Exp),
               reads=[t_rd], writes=[t_rd])
        sch.op("act", lambda: nc.scalar.activation(out=k.dec[:, 0:8], in_=rd[:, :], func=AF.Ln, scale=-1.0, bias=1.0),
               reads=[t_rd], writes=[k.t_dec])
        sch.op("act", lambda: nc.scalar.activation(out=k.dec[:, 8:16], in_=k.dec[:, 0:8], func=AF.Exp, scale=128.0),
               reads=[k.t_dec], writes=[k.t_dec])
        sch.op("dve", lambda: nc.vector.tensor_scalar(out=k.dec[:, 16:24], in0=k.dec[:, 0:8], scalar1=-1.0, scalar2=None,
                                                      op0=ALU.mult), reads=[k.t_dec], writes=[k.t_dec])
        if k.MOD is not None:
            sch.op("sp", lambda: nc.sync.dma_start(out=k.MOD[:, :], in_=k.modv[:, :]), reads=[k.t_modv], dma=True)
        if flush:
            sch.flush("A")


def phase_B(k):
    nc, sch = k.nc, k.sch
    with ExitStack() as st:
        sb, ps = k.sb, k.ps
        xt = [sb(st, f"xt{i}", [128, 8, 512], F32) for i in range(2)]
        sq = [sb(st, f"sq{i}", [128, 8, 512], F32) for i in range(2)]
        rs = [sb(st, f"rs{i}", [128, 512], F32) for i in range(2)]
        tmp = [sb(st, f"tmp{i}", [128, 512], F32) for i in range(2)]
        pss = [ps(st, f"pss{i}", [128, 512]) for i in range(2)]
        t_xt, t_sq, t_rs, t_pss = ([Tok(), Tok()] for _ in range(4))
        t_tmp = [Tok(), Tok()]
        xv = k.xT.rearrange("kk p t -> p kk t")
        for t in range(8):
            b = t % 2
            sch.op("sp", lambda t=t, b=b: nc.sync.dma_start(out=xt[b][:, :, :], in_=xv[:, :, t * 512:(t + 1) * 512]),
                   writes=[t_xt[b]], dma=True)
            sch.op("act", lambda b=b: nc.scalar.activation(out=sq[b][:, :, :], in_=xt[b][:, :, :], func=AF.Square),
                   reads=[t_xt[b]], writes=[t_sq[b]])
            for kk in range(8):
                sch.op("pe", lambda b=b, kk=kk: nc.tensor.matmul(
                    pss[b][:, :], lhsT=k.ones_f, rhs=sq[b][:, kk, :], start=(kk == 0), stop=(kk == 7)),
                    reads=[t_sq[b], k.t_cf], writes=[t_pss[b]])
            sch.op("act", lambda b=b: nc.scalar.activation(out=rs[b][:, :], in_=pss[b][:, :], func=AF.Sqrt,
                                                            scale=1.0 / D, bias=EPS),
                   reads=[t_pss[b]], writes=[t_rs[b]])
            sch.op("dve", lambda b=b: nc.vector.reciprocal(out=rs[b][:, :], in_=rs[b][:, :]),
                   reads=[t_rs[b]], writes=[t_rs[b]])
            for kk in range(8):
                tb = kk % 2
                sch.op("dve", lambda b=b, kk=kk, tb=tb: nc.vector.scalar_tensor_tensor(
                    out=tmp[tb][:, :], in0=xt[b][:, kk, :], scalar=k.modv[:, 48 + kk:49 + kk], in1=rs[b][:, :],
                    op0=ALU.mult, op1=ALU.mult),
                    reads=[t_xt[b], t_rs[b], k.t_modv], writes=[t_tmp[tb]])
                sch.op("act", lambda t=t, kk=kk, tb=tb: nc.scalar.activation(
                    out=k.hT[:, kk, t * 512:(t + 1) * 512], in_=tmp[tb][:, :], func=AF.Identity,
                    bias=k.modv[:, kk:kk + 1], scale=1.0),
                    reads=[t_tmp[tb], k.t_modv], writes=[k.t_hT[t]])
        if k.HT is not None:
            for kk in range(8):
                sch.op("sp", lambda kk=kk: nc.sync.dma_start(out=k.HT[kk, :, :], in_=k.hT[:, kk, :]),
                       reads=k.t_hT, dma=True)
        sch.flush("B")


def phase_C(k):
    nc, sch = k.nc, k.sch
    with ExitStack() as st:
        sb, ps = k.sb, k.ps
        wf = [sb(st, f"wf{i}", [128, 4, 512], F32) for i in range(2)]
        wb = [sb(st, f"wb{i}", [128, 8, 512], BF16) for i in range(2)]
        stg = [sb(st, f"stg{i}", [128, 2048], BF16) for i in range(8)]
        cs = [sb(st, f"cs{i}", [128, 512], F32) for i in range(2)]
        sn = [sb(st, f"sn{i}", [128, 512], F32) for i in range(2)]
        tabs = [sb(st, f"tab{i}", [128, 512], F32) for i in range(2)]
        ta = [sb(st, f"ta{i}", [128, 512], F32) for i in range(4)]
        o12 = [sb(st, f"o12{i}", [128, 512], F32) for i in range(2)]
        bank = [ps(st, f"bk{i}", [128, 512]) for i in range(8)]
        t_wf, t_wb = [Tok(), Tok()], [Tok(), Tok()]
        t_stg = [Tok() for _ in range(8)]
        t_cs, t_sn = [Tok(), Tok()], [Tok(), Tok()]
        t_tab = [Tok(), Tok()]
        t_ta = [Tok() for _ in range(4)]
        t_o12 = [Tok(), Tok()]
        t_bank = [Tok() for _ in range(8)]
        wv = k.w_in.rearrange("(kk p) n -> p kk n", p=128)
        cnt = {"bank": 0, "stg": 0, "cs": 0}

        def nbank():
            i = cnt["bank"] % 8
            cnt["bank"] += 1
            return i

        def nstg():
            i = cnt["stg"] % 8
            cnt["stg"] += 1
            return i

        def load_slab(s):
            b = s % 2
            for hlf in range(2):
                sch.op("sp", lambda s=s, hlf=hlf: nc.sync.dma_start(
                    out=wf[hlf][:, :, :], in_=wv[:, hlf * 4:(hlf + 1) * 4, s * 512:(s + 1) * 512]),
                    writes=[t_wf[hlf]], dma=True)
                sch.op("pool", lambda b=b, hlf=hlf: nc.gpsimd.tensor_copy(
                    out=wb[b][:, hlf * 4:(hlf + 1) * 4, :], in_=wf[hlf][:, :, :]),
                    reads=[t_wf[hlf]], writes=[t_wb[b]])

        def mm_feat(b, j, t, bi):
            for kk in range(8):
                sch.op("pe", lambda b=b, j=j, t=t, bi=bi, kk=kk: nc.tensor.matmul(
                    bank[bi][:, :], lhsT=wb[b][:, kk, j * 128:(j + 1) * 128], rhs=k.hT[:, kk, t * 512:(t + 1) * 512],
                    start=(kk == 0), stop=(kk == 7)),
                    reads=[t_wb[b], k.t_hT[t]], writes=[t_bank[bi]])

        load_slab(0)
        for s in range(18):
            b = s % 2
            if s + 1 < 18:
                load_slab(s + 1)
            typ = s // 2
            if typ in (0, 1):
                dstF, dstB = (k.QF, k.QB) if typ == 0 else (k.KF, k.KB)
                for hh in range(2):
                    head = (s % 2) * 2 + hh
                    for v in range(2):
                        col = v * 4 + head
                        if typ == 0:
                            sch.op("act", lambda v=v, col=col: nc.scalar.activation(
                                out=tabs[v][:, :], in_=(k.posF if v == 0 else k.posB), func=AF.Exp,
                                scale=k.dec[:, col:col + 1]),
                                reads=[k.t_dec, k.t_cf], writes=[t_tab[v]])
                        else:
                            sch.op("act", lambda v=v, col=col: nc.scalar.activation(
                                out=tabs[v][:, :], in_=(k.posF if v == 0 else k.posB), func=AF.Exp,
                                scale=k.dec[:, 16 + col:17 + col], bias=math.log(1.0 / 16.0)),
                                reads=[k.t_dec, k.t_cf], writes=[t_tab[v]])
                    for half in range(2):
                        sg = [nstg() for _ in range(4)]
                        for tt in range(4):
                            t = half * 4 + tt
                            ci = cnt["cs"] % 2
                            cnt["cs"] += 1
                            sch.op("sp", lambda t=t, ci=ci: nc.sync.dma_start(
                                out=cs[ci][:, :], in_=k.cosT[:, t * 512:(t + 1) * 512]), writes=[t_cs[ci]], dma=True)
                            sch.op("sp", lambda t=t, ci=ci: nc.sync.dma_start(
                                out=sn[ci][:, :], in_=k.sinT[:, t * 512:(t + 1) * 512]), writes=[t_sn[ci]], dma=True)
                            b1, b2 = nbank(), nbank()
                            mm_feat(b, hh * 2, t, b1)
                            mm_feat(b, hh * 2 + 1, t, b2)
                            TT = nc.vector.tensor_tensor
                            sch.op("dve", lambda b1=b1, ci=ci: TT(out=ta[0][:, :], in0=bank[b1][:, :], in1=cs[ci][:, :], op=ALU.mult),
                                   reads=[t_bank[b1], t_cs[ci]], writes=[t_ta[0]])
                            sch.op("dve", lambda b2=b2, ci=ci: TT(out=ta[1][:, :], in0=bank[b2][:, :], in1=sn[ci][:, :], op=ALU.mult),
                                   reads=[t_bank[b2], t_sn[ci]], writes=[t_ta[1]])
                            sch.op("dve", lambda b1=b1, ci=ci: TT(out=ta[2][:, :], in0=bank[b1][:, :], in1=sn[ci][:, :], op=ALU.mult),
                                   reads=[t_bank[b1], t_sn[ci]], writes=[t_ta[2]])
                            sch.op("dve", lambda b2=b2, ci=ci: TT(out=ta[3][:, :], in0=bank[b2][:, :], in1=cs[ci][:, :], op=ALU.mult),
                                   reads=[t_bank[b2], t_cs[ci]], writes=[t_ta[3]])
                            PT = nc.gpsimd.tensor_tensor
                            sch.op("dve", lambda: TT(out=o12[0][:, :], in0=ta[0][:, :], in1=ta[1][:, :], op=ALU.subtract),
                                   reads=[t_ta[0], t_ta[1]], writes=[t_o12[0]])
                            sch.op("dve", lambda: TT(out=o12[1][:, :], in0=ta[2][:, :], in1=ta[3][:, :], op=ALU.add),
                                   reads=[t_ta[2], t_ta[3]], writes=[t_o12[1]])
                            for v in range(2):
                                for c in range(2):
                                    si = sg[v * 2 + c]
                                    sch.op("pool", lambda v=v, c=c, si=si, tt=tt: PT(
                                        out=stg[si][:, tt * 512:(tt + 1) * 512], in0=o12[c][:, :], in1=tabs[v][:, :],
                                        op=ALU.mult),
                                        reads=[t_o12[c], t_tab[v]], writes=[t_stg[si]])
                        for v in range(2):
                            for c in range(2):
                                si = sg[v * 2 + c]
                                dst = dstF if v == 0 else dstB
                                sch.op("sp", lambda dst=dst, si=si, head=head, c=c, half=half: nc.sync.dma_start(
                                    out=dst[head * 2 + c, :, half * 2048:(half + 1) * 2048], in_=stg[si][:, :]),
                                    reads=[t_stg[si]], dma=True)
            elif typ in (2, 6):
                dst = k.RV if typ == 2 else k.AV
                c0 = (s % 2) * 512
                for t4 in range(8):
                    si = nstg()
                    for i in range(4):
                        t = t4 * 4 + i
                        bi = nbank()
                        for kk in range(8):
                            sch.op("pe", lambda b=b, t=t, bi=bi, kk=kk: nc.tensor.matmul(
                                bank[bi][:, :], lhsT=k.hT[:, kk, t * 128:(t + 1) * 128], rhs=wb[b][:, kk, :],
                                start=(kk == 0), stop=(kk == 7)),
                                reads=[t_wb[b], k.t_hT[t // 4]], writes=[t_bank[bi]])
                        if i % 2 == 0:
                            sch.op("act", lambda si=si, i=i, bi=bi: nc.scalar.copy(
                                out=stg[si][:, i * 512:(i + 1) * 512], in_=bank[bi][:, :]),
                                reads=[t_bank[bi]], writes=[t_stg[si]])
                        else:
                            sch.op("dve", lambda si=si, i=i, bi=bi: nc.vector.tensor_copy(
                                out=stg[si][:, i * 512:(i + 1) * 512], in_=bank[bi][:, :]),
                                reads=[t_bank[bi]], writes=[t_stg[si]])
                    dv = dst[t4 * 512:(t4 + 1) * 512, c0:c0 + 512].rearrange("(i p) c -> p i c", p=128)
                    sch.op("sp", lambda dv=dv, si=si: nc.sync.dma_start(
                        out=dv, in_=stg[si][:, :].rearrange("p (i c) -> p i c", i=4)),
                        reads=[t_stg[si]], dma=True)
            else:
                dst = {3: k.SG, 4: k.AQ, 5: k.AK, 7: k.GR, 8: k.GA}[typ]
                func = {3: AF.Silu, 4: AF.Copy, 5: AF.Copy, 7: AF.Sigmoid, 8: AF.Sigmoid}[typ]
                for j in range(4):
                    chunk = (s % 2) * 4 + j
                    for half in range(2):
                        si = nstg()
                        for tt in range(4):
                            t = half * 4 + tt
                            bi = nbank()
                            mm_feat(b, j, t, bi)
                            if func == AF.Copy and tt % 2 == 1:
                                sch.op("dve", lambda si=si, tt=tt, bi=bi: nc.vector.tensor_copy(
                                    out=stg[si][:, tt * 512:(tt + 1) * 512], in_=bank[bi][:, :]),
                                    reads=[t_bank[bi]], writes=[t_stg[si]])
                            else:
                                sch.op("act", lambda si=si, tt=tt, bi=bi, func=func: nc.scalar.activation(
                                    out=stg[si][:, tt * 512:(tt + 1) * 512], in_=bank[bi][:, :], func=func),
                                    reads=[t_bank[bi]], writes=[t_stg[si]])
                        sch.op("sp", lambda dst=dst, si=si, chunk=chunk, half=half: nc.sync.dma_start(
                            out=dst[chunk, :, half * 2048:(half + 1) * 2048], in_=stg[si][:, :]),
                            reads=[t_stg[si]], dma=True)
        sch.flush("C")


def _t5_buckets():
    out = np.zeros((3, 128, 384), np.int64)
    kk = np.arange(128)[:, None]
    for g, r in enumerate((1, 4, 16)):
        for t in (-1, 0, 1):
            q = np.arange(128)[None, :]
            rel = (t * 128 + kk - q) * r
            n = np.abs(rel)
            large = 8 + (np.log(np.maximum(n, 1).astype(np.float32) / np.float32(8))
                         / np.float32(math.log(1024 / 8)) * np.float32(8)).astype(np.int32)
            large = np.minimum(large, 15)
            bk = np.where(rel > 0, 16, 0) + np.where(n < 8, n, large)
            bk = np.where(np.abs(t * 128 + kk - q) <= 64, bk, 0)
            out[g, :, (t + 1) * 128:(t + 2) * 128] = bk
    return out


def _constants():
    c = {}
    half = 128
    inv_freq = (np.float32(10000.0) ** (-np.arange(half, dtype=np.float32) / np.float32(half))).astype(np.float32)
    ang = (np.arange(S, dtype=np.float32)[:, None] * inv_freq[None, :]).astype(np.float32)
    c["cosT"] = np.ascontiguousarray(np.cos(ang).astype(np.float32).T)
    c["sinT"] = np.ascontiguousarray(np.sin(ang).astype(np.float32).T)
    cf = np.zeros((128, 1280), np.float32)
    cf[:, 0:128] = np.eye(128, dtype=np.float32)
    cf[:, 128:256] = 1.0
    i = np.arange(128, dtype=np.float32)
    cf[:, 256:768] = np.tile(i + 1.0, 4)[None, :]
    cf[:, 768:1280] = np.tile(128.0 - i, 4)[None, :]
    c["cst_f32"] = cf
    cb = np.zeros((128, 1024), np.float32)
    cb[:, 896:1024] = 1.0
    cb[:, 0:128] = np.eye(128)
    jj = np.arange(128)[:, None]
    ii = np.arange(128)[None, :]
    cb[:, 128:256] = (jj <= ii)
    cb[:, 256:384] = (jj > ii)
    cb[:, 384:512] = (jj < ii)
    for t in (-1, 0, 1):
        cb[:, 512 + (t + 1) * 128:512 + (t + 2) * 128] = (np.abs(t * 128 + jj - ii) <= 64)
    c["cst_bf"] = _bf16(cb)
    cg = np.zeros((128, 1024), np.float32)
    cg[:, 0:512] = np.arange(512, dtype=np.float32)[None, :]
    cg[:, 512:768] = (np.arange(NE, dtype=np.float32) + 1.0)[None, :]
    cg[:, 768] = np.arange(128, dtype=np.float32)
    c["cst_g"] = cg
    return c


def _shared_inputs(inp, upto="H"):
    m = dict(_constants())
    f = lambda a: np.ascontiguousarray(np.asarray(a, dtype=np.float32))
    m["w_ada"] = f(inp["w_ada"][0])
    m["b_ada_l"] = f(inp["b_ada"][0].reshape(48, 128).T)
    m["b_gf_bc"] = f(np.tile(np.asarray(inp["b_ada"])[0, 5 * D:6 * D][None, :], (128, 1)))
    m["nmix_l"] = f(inp["norm_mix"][0].reshape(8, 128).T)
    m["nffn_l"] = f(inp["norm_ffn"][0].reshape(8, 128).T)
    m["w_in"] = f(inp["w_in"][0])
    m["rdecay_bc"] = f(np.tile(np.asarray(inp["ret_decay"])[0].reshape(1, 8), (128, 1)))
    bk = _t5_buckets()
    t5 = np.asarray(inp["t5_bias"], dtype=np.float32)
    tb = t5[bk]
    tb = tb.reshape(3, 128, 384, 8, 2).transpose(3, 1, 0, 4, 2)
    m["t5b"] = f(tb.reshape(8, 128, 6 * 384))
    m["w_ret_up"] = f(inp["w_ret_up"][0])
    m["w_att_up"] = f(inp["w_att_up"][0])
    m["w_o"] = f(inp["w_o"][0])
    m["w_router"] = f(inp["w_router"][0])
    m["rbias_bc"] = f(np.tile(np.asarray(inp["router_bias"])[0][None, :], (128, 1)))
    if upto >= "G":
        wa = np.empty((NE, 128, 3, 2048), np.float32)
        wa[:, :, 0, :] = np.asarray(inp["w_gate"][0]).reshape(NE, 8, 128, 256).transpose(0, 2, 1, 3).reshape(NE, 128, 2048)
        wa[:, :, 1, :] = np.asarray(inp["w_up"][0]).reshape(NE, 8, 128, 256).transpose(0, 2, 1, 3).reshape(NE, 128, 2048)
        wa[:, :, 2, :] = np.asarray(inp["w_down"][0]).reshape(NE, 2, 128, D).transpose(0, 2, 1, 3).reshape(NE, 128, 2048)
        m["w_all"] = wa.reshape(NE * 128, 6144)
    m["ws_gate"] = f(inp["ws_gate"][0])
    m["ws_up"] = f(inp["ws_up"][0])
    m["ws_down"] = f(inp["ws_down"][0])
    m["nfin_bc"] = f(np.tile(np.asarray(inp["norm_final"])[None, :], (128, 1)))
    return m


def _core_inputs(inp, b, shared):
    m = dict(shared)
    x = np.asarray(inp["x"][b], dtype=np.float32)
    m["xT"] = np.ascontiguousarray(x.T).reshape(8, 128, S)
    m["c_l"] = np.ascontiguousarray(np.asarray(inp["c"][b], dtype=np.float32).reshape(8, 128).T)
    return m


_NC_CACHE = {}


def kernel(**inputs):
    if "nc" not in _NC_CACHE:
        _NC_CACHE["nc"] = build_nc()
    nc = _NC_CACHE["nc"]
    shared = _shared_inputs(inputs)
    in_maps = [_core_inputs(inputs, b, shared) for b in range(8)]
    res = run_bass_kernel_spmd(nc, in_maps, core_ids=list(range(8)))
    return np.stack([np.asarray(r["out"], dtype=np.float32) for r in res.results], axis=0)


def phase_D(k):
    nc, sch = k.nc, k.sch
    with ExitStack() as st:
        sb, ps = k.sb, k.ps
        qf = sb(st, "qf", [128, 2, S], BF16)
        qb = sb(st, "qb", [128, 2, S], BF16)
        kf = sb(st, "kf", [128, 2, S], BF16)
        kb = sb(st, "kb", [128, 2, S], BF16)
        sg = sb(st, "sg", [128, 2, S], BF16)
        vv = sb(st, "vv", [128, NT, 256], BF16)
        sbs = sb(st, "sbs", [128, NT, 2, 256], BF16)
        oret = sb(st, "oret", [128, 2, S], BF16)
        Sm = sb(st, "Sm", [128, 2, 256], F32)
        Tm = sb(st, "Tm", [128, 2, 256], F32)
        sfb = [sb(st, f"sfb{i}", [128, 2, 256], BF16) for i in range(2)]
        kt = [sb(st, f"kt{i}", [128, 256], BF16) for i in range(2)]
        t1 = [sb(st, f"rt1{i}", [128, 128], BF16) for i in range(2)]
        t2 = [sb(st, f"rt2{i}", [128, 128], BF16) for i in range(2)]
        pt = [sb(st, f"rpt{i}", [128, 128], BF16) for i in range(2)]
        og = [sb(st, f"og{i}", [128, 2, 256], F32) for i in range(2)]
        osq = sb(st, "osq", [128, 2, 256], F32)
        mean = sb(st, "mean", [128, 256], F32)
        msq = sb(st, "msq", [128, 256], F32)
        rstd = sb(st, "rstd", [128, 256], F32)
        tn = [sb(st, f"tn{i}", [128, 256], F32) for i in range(2)]
        ptr = [ps(st, f"ptr{i}", [128, 256], BF16) for i in range(2)]
        pds = ps(st, "pds", [128, 2, 256])
        pS = [ps(st, f"pS{i}", [128, 256]) for i in range(2)]
        pO = [ps(st, f"pO{i}", [128, 2, 128]) for i in range(2)]
        pst = ps(st, "pst", [128, 2, 256])
        T = Tok
        t_qf, t_qb, t_kf, t_kb, t_sg, t_vv, t_oret, t_Sm, t_Tm = (T() for _ in range(9))
        t_sbs = [T() for _ in range(NT)]
        t_sfb, t_kt, t_t1, t_t2, t_pt, t_og = ([T(), T()] for _ in range(6))
        t_osq, t_mean, t_msq, t_rstd = T(), T(), T(), T()
        t_tn = [T(), T()]
        t_ptr, t_pS, t_pO = [T(), T()], [T(), T()], [T(), T()]
        t_pds, t_pst = T(), T()
        RVv = k.RV.rearrange("(n p) c -> p n c", p=128)
        cnt = {"kt": 0}

        def ktrans(src, t_src, n):
            i = cnt["kt"] % 2
            cnt["kt"] += 1
            for dc in range(2):
                sch.op("pe", lambda i=i, dc=dc, n=n: nc.tensor.transpose(
                    out=ptr[i][:, dc * 128:(dc + 1) * 128], in_=src[:, dc, n * 128:(n + 1) * 128], identity=k.ident_b),
                    reads=[t_src, k.t_cb], writes=[t_ptr[i]])
            sch.op("act", lambda i=i: nc.scalar.copy(out=kt[i][:, :], in_=ptr[i][:, :]),
                   reads=[t_ptr[i]], writes=[t_kt[i]])
            return i

        def dstate(i, n):
            for dc in range(2):
                sch.op("pe", lambda i=i, dc=dc, n=n: nc.tensor.matmul(
                    pds[:, dc, :], lhsT=kt[i][:, dc * 128:(dc + 1) * 128], rhs=vv[:, n, :], start=True, stop=True),
                    reads=[t_kt[i], t_vv], writes=[t_pds])

        def supdate(first, cd, bf_out, t_bf, cast_eng="act"):
            if first:
                sch.op("dve", lambda: nc.vector.tensor_scalar(
                    out=Sm[:, :, :], in0=pds[:, :, :], scalar1=cd, scalar2=None, op0=ALU.mult),
                    reads=[t_pds, k.t_dec], writes=[t_Sm])
            else:
                sch.op("dve", lambda: nc.vector.scalar_tensor_tensor(
                    out=Sm[:, :, :], in0=pds[:, :, :], scalar=cd, in1=Tm[:, :, :], op0=ALU.mult, op1=ALU.add),
                    reads=[t_pds, t_Tm, k.t_dec], writes=[t_Sm])
            sch.op("act", lambda: nc.scalar.activation(out=Tm[:, :, :], in_=Sm[:, :, :], func=AF.Copy, scale=cd),
                   reads=[t_Sm, k.t_dec], writes=[t_Tm])
            if cast_eng == "act":
                sch.op("act", lambda: nc.scalar.copy(out=bf_out, in_=Sm[:, :, :]), reads=[t_Sm], writes=[t_bf])
            else:
                sch.op("pool", lambda: nc.gpsimd.tensor_copy(out=bf_out, in_=Sm[:, :, :]), reads=[t_Sm], writes=[t_bf])

        for h in range(4):
            sch.op("sp", lambda h=h: nc.sync.dma_start(
                out=kb[:, :, :], in_=k.KB[2 * h:2 * h + 2, :, :].rearrange("c p t -> p c t")), writes=[t_kb], dma=True)
            sch.op("sp", lambda h=h: nc.sync.dma_start(out=vv[:, :, :], in_=RVv[:, :, 256 * h:256 * h + 256]),
                   writes=[t_vv], dma=True)
            for (buf, tk, src) in ((kf, t_kf, k.KF), (qf, t_qf, k.QF), (qb, t_qb, k.QB), (sg, t_sg, k.SG)):
                sch.op("sp", lambda buf=buf, src=src, h=h: nc.sync.dma_start(
                    out=buf[:, :, :], in_=src[2 * h:2 * h + 2, :, :].rearrange("c p t -> p c t")),
                    writes=[tk], dma=True)
            cdF = k.dec[:, 8 + h:9 + h]
            cdB = k.dec[:, 12 + h:13 + h]
            inext = ktrans(kb, t_kb, NT - 1)
            for n in range(NT - 1, 0, -1):
                i = inext
                if n - 1 >= 1:
                    inext = ktrans(kb, t_kb, n - 1)
                dstate(i, n)
                supdate(n == NT - 1, cdB, sbs[:, n - 1, :, :], t_sbs[n - 1], cast_eng="pool")

            def s_part(n):
                p2 = n % 2
                c0, c1 = n * 128, (n + 1) * 128
                for (col, ksrc, tks, qsrc, tqs) in ((0, kf, t_kf, qf, t_qf), (1, kb, t_kb, qb, t_qb)):
                    for dc in range(2):
                        sch.op("pe", lambda p2=p2, col=col, ksrc=ksrc, qsrc=qsrc, dc=dc, c0=c0, c1=c1: nc.tensor.matmul(
                            pS[p2][:, col * 128:(col + 1) * 128], lhsT=ksrc[:, dc, c0:c1], rhs=qsrc[:, dc, c0:c1],
                            start=(dc == 0), stop=(dc == 1)),
                            reads=[tks, tqs], writes=[t_pS[p2]])
                sch.op("dve", lambda p2=p2: nc.vector.tensor_tensor(
                    out=t1[p2][:, :], in0=pS[p2][:, 0:128], in1=k.maskF, op=ALU.mult),
                    reads=[t_pS[p2], k.t_cb], writes=[t_t1[p2]])
                sch.op("dve", lambda p2=p2: nc.vector.tensor_tensor(
                    out=t2[p2][:, :], in0=pS[p2][:, 128:256], in1=k.maskB, op=ALU.mult),
                    reads=[t_pS[p2], k.t_cb], writes=[t_t2[p2]])
                sch.op("dve", lambda p2=p2: nc.vector.tensor_tensor(
                    out=pt[p2][:, :], in0=t1[p2][:, :], in1=t2[p2][:, :], op=ALU.add),
                    reads=[t_t1[p2], t_t2[p2]], writes=[t_pt[p2]])

            s_part(0)
            ki = ktrans(kf, t_kf, 0)
            for n in range(NT):
                p2 = n % 2
                c0, c1 = n * 128, (n + 1) * 128
                ki_cur = ki
                if n + 1 < NT:
                    s_part(n + 1)
                    if n + 1 < NT - 1:
                        ki = ktrans(kf, t_kf, n + 1)
                if n < NT - 1:
                    dstate(ki_cur, n)
                sfi = n % 2
                for ec in range(2):
                    e0, e1 = ec * 128, (ec + 1) * 128
                    mms = [(vv[:, n, e0:e1], pt[p2][:, :], [t_vv, t_pt[p2]])]
                    if n > 0:
                        for dc in range(2):
                            mms.append((sfb[sfi][:, dc, e0:e1], qf[:, dc, c0:c1], [t_sfb[sfi], t_qf]))
                    if n < NT - 1:
                        for dc in range(2):
                            mms.append((sbs[:, n, dc, e0:e1], qb[:, dc, c0:c1], [t_sbs[n], t_qb]))
                    for mi, (lt, rh, rd) in enumerate(mms):
                        sch.op("pe", lambda p2=p2, ec=ec, lt=lt, rh=rh, mi=mi, last=(mi == len(mms) - 1): nc.tensor.matmul(
                            pO[p2][:, ec, :], lhsT=lt, rhs=rh, start=(mi == 0), stop=last),
                            reads=rd, writes=[t_pO[p2]])
                if n < NT - 1:
                    supdate(n == 0, cdF, sfb[(n + 1) % 2][:, :, :], t_sfb[(n + 1) % 2])
                gi = (n // 2) % 2
                gp = n % 2
                sch.op("act", lambda p2=p2, gi=gi, gp=gp: nc.scalar.copy(
                    out=og[gi][:, :, gp * 128:(gp + 1) * 128], in_=pO[p2][:, :, :]),
                    reads=[t_pO[p2]], writes=[t_og[gi]])
                if gp == 1:
                    g0 = (n - 1) * 128
                    sch.op("act", lambda gi=gi: nc.scalar.activation(out=osq[:, :, :], in_=og[gi][:, :, :], func=AF.Square),
                           reads=[t_og[gi]], writes=[t_osq])
                    for (sidx, src, tsrc) in ((0, og[gi], t_og[gi]), (1, osq, t_osq)):
                        for ec in range(2):
                            sch.op("pe", lambda sidx=sidx, src=src, ec=ec: nc.tensor.matmul(
                                pst[:, sidx, :], lhsT=k.ones_f, rhs=src[:, ec, :], start=(ec == 0), stop=(ec == 1)),
                                reads=[tsrc, k.t_cf], writes=[t_pst])
                    sch.op("act", lambda: nc.scalar.activation(out=mean[:, :], in_=pst[:, 0, :], func=AF.Copy, scale=1.0 / 256),
                           reads=[t_pst], writes=[t_mean])
                    sch.op("dve", lambda: nc.vector.tensor_tensor(out=msq[:, :], in0=mean[:, :], in1=mean[:, :], op=ALU.mult),
                           reads=[t_mean], writes=[t_msq])
                    sch.op("dve", lambda: nc.vector.scalar_tensor_tensor(
                        out=rstd[:, :], in0=pst[:, 1, :], scalar=1.0 / 256, in1=msq[:, :], op0=ALU.mult, op1=ALU.subtract),
                        reads=[t_pst, t_msq], writes=[t_rstd])
                    sch.op("act", lambda: nc.scalar.activation(out=rstd[:, :], in_=rstd[:, :], func=AF.Sqrt, bias=EPS, scale=1.0),
                           reads=[t_rstd], writes=[t_rstd])
                    sch.op("dve", lambda: nc.vector.reciprocal(out=rstd[:, :], in_=rstd[:, :]),
                           reads=[t_rstd], writes=[t_rstd])
                    for ec in range(2):
                        sch.op("pool", lambda gi=gi, ec=ec: nc.gpsimd.tensor_tensor(
                            out=tn[ec][:, :], in0=og[gi][:, ec, :], in1=mean[:, :], op=ALU.subtract),
                            reads=[t_og[gi], t_mean], writes=[t_tn[ec]])
                        sch.op("pool", lambda ec=ec: nc.gpsimd.tensor_tensor(
                            out=tn[ec][:, :], in0=tn[ec][:, :], in1=rstd[:, :], op=ALU.mult),
                            reads=[t_tn[ec], t_rstd], writes=[t_tn[ec]])
                        sch.op("dve", lambda ec=ec, g0=g0: nc.vector.tensor_tensor(
                            out=oret[:, ec, g0:g0 + 256], in0=tn[ec][:, :], in1=sg[:, ec, g0:g0 + 256], op=ALU.mult),
                            reads=[t_tn[ec], t_sg], writes=[t_oret])
            for ec in range(2):
                sch.op("sp", lambda h=h, ec=ec: nc.sync.dma_start(out=k.ORET[2 * h + ec, :, :], in_=oret[:, ec, :]),
                       reads=[t_oret], dma=True)
        sch.flush("D")


def phase_E(k):
    nc, sch = k.nc, k.sch
    with ExitStack() as st:
        sb, ps = k.sb, k.ps
        aq = [sb(st, f"aq{i}", [128, S], BF16) for i in range(2)]
        ak = [sb(st, f"ak{i}", [128, S], BF16) for i in range(2)]
        bst = sb(st, "bst", [128, 6, 384], F32)
        eb = [sb(st, f"eb{i}", [128, 6, 384], BF16) for i in range(2)]
        vg = [sb(st, f"vg{i}", [128, NT, 128], BF16) for i in range(2)]
        accn = sb(st, "accn", [128, S], F32)
        accz = sb(st, "accz", [128, S], F32)
        ost = sb(st, "ost", [128, S], BF16)
        esb = [sb(st, f"esb{i}", [128, 384], BF16) for i in range(4)]
        ptb = [sb(st, f"ptb{i}", [128, 384], BF16) for i in range(4)]
        pS = [ps(st, f"apS{i}", [128, 512]) for i in range(4)]
        pO = [ps(st, f"apO{i}", [128, 128]) for i in range(2)]
        pZ = [ps(st, f"apZ{i}", [128, 128]) for i in range(2)]
        T = Tok
        t_bst, t_accn, t_accz, t_ost = (T() for _ in range(4))
        t_aq, t_ak, t_eb, t_vg = ([T(), T()] for _ in range(4))
        t_esb = [T() for _ in range(4)]
        t_ptb = [T() for _ in range(4)]
        t_pS = [T() for _ in range(4)]
        t_pO, t_pZ = [T(), T()], [T(), T()]
        ones_b = k.cb[:, 896:960]
        RS = (1, 4, 16)
        groups = [(hp, g) for hp in range(8) for g in range(3)]

        def load_group(gi):
            hp, g = groups[gi]
            hb = hp % 2
            if g == 0:
                sch.op("sp", lambda: nc.sync.dma_start(out=aq[hb][:, :], in_=k.AQ[hp, :, :]), writes=[t_aq[hb]], dma=True)
                sch.op("sp", lambda: nc.sync.dma_start(out=ak[hb][:, :], in_=k.AK[hp, :, :]), writes=[t_ak[hb]], dma=True)
                sch.op("sp", lambda: nc.sync.dma_start(
                    out=bst[:, :, :], in_=k.t5b[hp, :, :].rearrange("p (a n) -> p a n", a=6)), writes=[t_bst], dma=True)
                sch.op("act", lambda: nc.scalar.activation(out=bst[:, :, :], in_=bst[:, :, :], func=AF.Exp),
                       reads=[t_bst], writes=[t_bst])
                for a in range(6):
                    sch.op("dve", lambda a=a: nc.vector.tensor_tensor(
                        out=eb[hb][:, a, :], in0=bst[:, a, :], in1=k.attmask, op=ALU.mult),
                        reads=[t_bst, k.t_cb], writes=[t_eb[hb]])
            r = RS[g]
            nb = NT // r
            vi = gi % 2
            src = k.AV[:, 128 * hp:128 * hp + 128].rearrange("(b kk c) f -> kk c b f", kk=128, c=r)
            for c in range(r):
                sch.op("sp", lambda c=c: nc.sync.dma_start(
                    out=vg[vi][:, c * nb:(c + 1) * nb, :], in_=src[:, c, :, :]), writes=[t_vg[vi]], dma=True)

        blocks = []
        for gi, (hp, g) in enumerate(groups):
            r = RS[g]
            nb = NT // r
            for c in range(r):
                for b in range(nb):
                    blocks.append(dict(gi=gi, hp=hp, g=g, r=r, nb=nb, c=c, b=b, first=(c == 0 and b == 0),
                                       last=(g == 2 and c == r - 1 and b == nb - 1)))

        def s_part(i):
            bl = blocks[i]
            hp, g, r, nb, c, b = bl["hp"], bl["g"], bl["r"], bl["nb"], bl["c"], bl["b"]
            hb = hp % 2
            aqv = aq[hb][:, :].rearrange("p (m r) -> p r m", r=r)
            akv = ak[hb][:, :].rearrange("p (m r) -> p r m", r=r)
            tl = [t for t in (-1, 0, 1) if 0 <= b + t < nb]
            cmin, cmax = (tl[0] + 1) * 128, (tl[-1] + 2) * 128
            for hh in range(2):
                si = (i % 2) * 2 + hh
                r0, r1 = 64 * hh, 64 * hh + 64
                for t in tl:
                    sch.op("pe", lambda si=si, t=t, r0=r0, r1=r1: nc.tensor.matmul(
                        pS[si][:, (t + 1) * 128:(t + 2) * 128],
                        lhsT=akv[r0:r1, c, 128 * (b + t):128 * (b + t + 1)],
                        rhs=aqv[r0:r1, c, 128 * b:128 * (b + 1)], start=True, stop=True),
                        reads=[t_ak[hb], t_aq[hb]], writes=[t_pS[si]])
                sch.op("act", lambda si=si: nc.scalar.activation(
                    out=esb[si][:, cmin:cmax], in_=pS[si][:, cmin:cmax], func=AF.Exp, scale=0.125),
                    reads=[t_pS[si]], writes=[t_esb[si]])
                sch.op("dve", lambda si=si, a=g * 2 + hh: nc.vector.tensor_tensor(
                    out=ptb[si][:, cmin:cmax], in0=esb[si][:, cmin:cmax], in1=eb[hb][:, a, cmin:cmax], op=ALU.mult),
                    reads=[t_esb[si], t_eb[hb]], writes=[t_ptb[si]])

        def pv_part(i):
            bl = blocks[i]
            hp, g, r, nb, c, b, gi = bl["hp"], bl["g"], bl["r"], bl["nb"], bl["c"], bl["b"], bl["gi"]
            vi = gi % 2
            tl = [t for t in (-1, 0, 1) if 0 <= b + t < nb]
            oi = i % 2
            for hh in range(2):
                si = (i % 2) * 2 + hh
                r0, r1 = 64 * hh, 64 * hh + 64
                for ti, t in enumerate(tl):
                    sch.op("pe", lambda si=si, t=t, r0=r0, r1=r1, blk=c * nb + b + t, ti=ti, nt=len(tl): nc.tensor.matmul(
                        pO[oi][r0:r1, :], lhsT=vg[vi][:, blk, r0:r1], rhs=ptb[si][:, (t + 1) * 128:(t + 2) * 128],
                        start=(ti == 0), stop=(ti == nt - 1)),
                        reads=[t_vg[vi], t_ptb[si]], writes=[t_pO[oi]])
                for ti, t in enumerate(tl):
                    sch.op("pe", lambda si=si, t=t, r0=r0, r1=r1, ti=ti, nt=len(tl): nc.tensor.matmul(
                        pZ[oi][r0:r1, :], lhsT=ones_b, rhs=ptb[si][:, (t + 1) * 128:(t + 2) * 128],
                        start=(ti == 0), stop=(ti == nt - 1)),
                        reads=[k.t_cb, t_ptb[si]], writes=[t_pZ[oi]])
            dn = accn[:, :].rearrange("p (m r) -> p r m", r=r)[:, c, 128 * b:128 * (b + 1)]
            dz = accz[:, :].rearrange("p (m r) -> p r m", r=r)[:, c, 128 * b:128 * (b + 1)]
            if g == 0:
                sch.op("act", lambda: nc.scalar.copy(out=dn, in_=pO[oi][:, :]), reads=[t_pO[oi]], writes=[t_accn])
                sch.op("act", lambda: nc.scalar.copy(out=dz, in_=pZ[oi][:, :]), reads=[t_pZ[oi]], writes=[t_accz])
            else:
                sch.op("dve", lambda: nc.vector.tensor_tensor(out=dn, in0=pO[oi][:, :], in1=dn, op=ALU.add),
                       reads=[t_pO[oi], t_accn], writes=[t_accn])
                sch.op("dve", lambda: nc.vector.tensor_tensor(out=dz, in0=pZ[oi][:, :], in1=dz, op=ALU.add),
                       reads=[t_pZ[oi], t_accz], writes=[t_accz])
            if bl["last"]:
                for q4 in range(4):
                    sl = slice(q4 * 1024, (q4 + 1) * 1024)
                    sch.op("dve", lambda sl=sl: nc.vector.reciprocal(out=accz[:, sl], in_=accz[:, sl]),
                           reads=[t_accz], writes=[t_accz])
                    sch.op("pool", lambda sl=sl: nc.gpsimd.tensor_tensor(out=ost[:, sl], in0=accn[:, sl], in1=accz[:, sl], op=ALU.mult),
                           reads=[t_accn, t_accz], writes=[t_ost])
                sch.op("sp", lambda: nc.sync.dma_start(out=k.OATT[hp, :, :], in_=ost[:, :]), reads=[t_ost], dma=True)

        load_group(0)
        load_group(1)
        s_part(0)
        for i in range(len(blocks)):
            if i + 1 < len(blocks):
                s_part(i + 1)
            pv_part(i)
            if i + 1 < len(blocks):
                nb_ = blocks[i + 1]
                if nb_["first"] and nb_["gi"] + 1 < len(groups):
                    load_group(nb_["gi"] + 1)
        sch.flush("E")


def phase_F0(k, st):
    nc, sch = k.nc, k.sch
    sb = k.sb
    k.wru = sb(st, "wru", [128, 8, D], BF16)
    k.wau = sb(st, "wau", [128, 8, D], BF16)
    k.wo = sb(st, "wo", [128, 8, D], BF16)
    k.wsg = sb(st, "wsg", [128, 8, 256], BF16)
    k.wsu = sb(st, "wsu", [128, 8, 256], BF16)
    k.wsd = sb(st, "wsd", [128, 2, D], BF16)
    k.wr = sb(st, "wr", [128, 8, NE], F32)
    k.rbias = sb(st, "rbias", [128, NE], F32)
    k.base = sb(st, "base", [128, NE], F32)
    k.cg = sb(st, "cg", [128, 1024], F32)
    k.t_cg = Tok()
    k.jrow = k.cg[:, 0:512]
    k.eplus1 = k.cg[:, 512:768]
    k.pcol = k.cg[:, 768:769]
    k.t_wF = Tok()
    k.t_base = Tok()
    with ExitStack() as s2:
        stg = [sb(s2, f"wstg{i}", [128, 4, D], F32) for i in range(2)]
        t_stg = [Tok(), Tok()]
        jobs = []
        for (dst, src) in ((k.wru, k.w_ret_up), (k.wau, k.w_att_up), (k.wo, k.w_o)):
            sv = src.rearrange("(kk p) n -> p kk n", p=128)
            for hlf in range(2):
                jobs.append((dst[:, hlf * 4:(hlf + 1) * 4, :], sv[:, hlf * 4:(hlf + 1) * 4, :], None))
        for (dst, src) in ((k.wsg, k.ws_gate), (k.wsu, k.ws_up)):
            sv = src.rearrange("(kk p) n -> p kk n", p=128)
            jobs.append((dst[:, :, :], sv[:, :, :], (8, 256)))
        jobs.append((k.wsd[:, :, :], k.ws_down.rearrange("(kk p) n -> p kk n", p=128), (2, D)))
        for ji, (dst, src, shp) in enumerate(jobs):
            b = ji % 2
            if shp is None:
                sview = stg[b][:, :, :]
            elif shp == (8, 256):
                sview = stg[b][:, :, :].rearrange("p a (b c) -> p (a b) c", c=256)[:, 0:8, :]
            else:
                sview = stg[b][:, 0:2, :]
            sch.op("sp", lambda sview=sview, src=src: nc.sync.dma_start(out=sview, in_=src), writes=[t_stg[b]], dma=True)
            eng = ("pool", "act", "dve")[ji % 3]
            if eng == "pool":
                sch.op("pool", lambda dst=dst, sview=sview: nc.gpsimd.tensor_copy(out=dst, in_=sview), reads=[t_stg[b]], writes=[k.t_wF])
            elif eng == "act":
                sch.op("act", lambda dst=dst, sview=sview: nc.scalar.copy(out=dst, in_=sview), reads=[t_stg[b]], writes=[k.t_wF])
            else:
                sch.op("dve", lambda dst=dst, sview=sview: nc.vector.tensor_copy(out=dst, in_=sview), reads=[t_stg[b]], writes=[k.t_wF])
        sch.op("sp", lambda: nc.sync.dma_start(out=k.wr[:, :, :], in_=k.w_router.rearrange("(kk p) n -> p kk n", p=128)),
               writes=[k.t_wF], dma=True)
        sch.op("sp", lambda: nc.sync.dma_start(out=k.rbias[:, :], in_=k.rbias_bc[:, :]), writes=[k.t_wF], dma=True)
        sch.op("pool", lambda: nc.gpsimd.memset(k.base[:, :], 0.0), writes=[k.t_base])
        sch.op("sp", lambda: nc.sync.dma_start(out=k.cg[:, :], in_=k.cst_g[:, :]), writes=[k.t_cg], dma=True)
        sch.flush("F0")


def phase_F1(k):
    nc, sch = k.nc, k.sch
    with ExitStack() as st:
        sb, ps = k.sb, k.ps
        oret_t = sb(st, "oret_t", [128, 8, 512], BF16)
        oatt_t = sb(st, "oatt_t", [128, 8, 512], BF16)
        gr_t = sb(st, "gr_t", [128, 8, 512], BF16)
        ga_t = sb(st, "ga_t", [128, 8, 512], BF16)
        x_t = sb(st, "x_t", [128, 8, 512], F32)
        merged = sb(st, "merged", [128, 8, 512], BF16)
        x1T = x_t
        h2T = sb(st, "h2T", [128, 8, 512], F32)
        h2Tb = sb(st, "h2Tb", [128, 8, 512], BF16)
        rs = sb(st, "rsF", [128, 512], F32)
        tA = [sb(st, f"tA{i}", [128, 512], F32) for i in range(2)]
        tB = [sb(st, f"tB{i}", [128, 512], F32) for i in range(2)]
        hid = sb(st, "hid", [128, 2, 512], BF16)
        x1tok = [sb(st, f"x1tok{i}", [128, D], F32) for i in range(1)] * 2
        h2tok = [sb(st, f"h2tok{i}", [128, D], BF16) for i in range(2)]
        xs = [sb(st, f"xs{i}", [128, D], F32) for i in range(1)] * 2
        sc = sb(st, "sc", [128, NE], F32)
        ch = sb(st, "ch", [128, NE], F32)
        chm = sb(st, "chm", [128, NE], F32)
        sel = sb(st, "sel", [128, NE], F32)
        selb = sb(st, "selb", [128, NE], BF16)
        gsel = sb(st, "gsel", [128, NE], F32)
        sval = sb(st, "sval", [128, NE], F32)
        junk = sb(st, "junk", [128, NE], F32)
        m8 = sb(st, "m8", [128, 8, 8], F32)
        sm = sb(st, "sm", [128, 64], F32)
        banks = [ps(st, f"fb{i}", [128, 512]) for i in range(7)]
        pH = ps(st, "fpH", [128, D], BF16)
        T = Tok
        t_oret, t_oatt, t_gr, t_ga, t_x, t_merged, t_x1T, t_h2T, t_h2Tb, t_rs, t_hid = (T() for _ in range(11))
        t_x1T = t_x
        t_tA, t_tB = [T(), T()], [T(), T()]
        t_x1tok, t_h2tok, t_xs = [T()] * 2, [T(), T()], [T()] * 2
        t_sc, t_ch, t_chm, t_sel, t_selb, t_gsel, t_sval, t_junk, t_m8, t_sm = (T() for _ in range(10))
        t_banks = [T() for _ in range(7)]
        t_pH = T()
        cnt = {"b": 0, "tA": 0, "tB": 0, "s": 0}

        def nbank():
            i = cnt["b"] % 7
            cnt["b"] += 1
            return i

        def fm(n_, c0):
            return n_[:, :, c0:c0 + 512].rearrange("c p t -> p c t")

        pending = []
        for Tt in range(8):
            c0 = Tt * 512
            for (buf, tk, src) in ((oret_t, t_oret, k.ORET), (oatt_t, t_oatt, k.OATT), (gr_t, t_gr, k.GR),
                                   (ga_t, t_ga, k.GA), (x_t, t_x, k.xT)):
                sch.op("sp", lambda buf=buf, src=src, c0=c0: nc.sync.dma_start(out=buf[:, :, :], in_=fm(src, c0)),
                       writes=[tk], dma=True)
            for n_ in range(8):
                n0, n1 = n_ * 128, (n_ + 1) * 128
                b1, b2 = nbank(), nbank()
                for (bi, w, act, tact) in ((b1, k.wru, oret_t, t_oret), (b2, k.wau, oatt_t, t_oatt)):
                    for kk in range(8):
                        sch.op("pe", lambda bi=bi, w=w, act=act, kk=kk, n0=n0, n1=n1: nc.tensor.matmul(
                            banks[bi][:, :], lhsT=w[:, kk, n0:n1], rhs=act[:, kk, :], start=(kk == 0), stop=(kk == 7)),
                            reads=[k.t_wF, tact], writes=[t_banks[bi]])
                ia = cnt["tA"] % 2
                cnt["tA"] += 1
                sch.op("dve", lambda b1=b1, ia=ia, n_=n_: nc.vector.tensor_tensor(
                    out=tA[ia][:, :], in0=banks[b1][:, :], in1=gr_t[:, n_, :], op=ALU.mult),
                    reads=[t_banks[b1], t_gr], writes=[t_tA[ia]])
                sch.op("dve", lambda b2=b2, ia=ia, n_=n_: nc.vector.tensor_tensor(
                    out=tB[ia][:, :], in0=banks[b2][:, :], in1=ga_t[:, n_, :], op=ALU.mult),
                    reads=[t_banks[b2], t_ga], writes=[t_tB[ia]])
                sch.op("pool", lambda ia=ia, n_=n_: nc.gpsimd.tensor_tensor(
                    out=merged[:, n_, :], in0=tA[ia][:, :], in1=tB[ia][:, :], op=ALU.add),
                    reads=[t_tA[ia], t_tB[ia]], writes=[t_merged])
            for n_ in range(8):
                n0, n1 = n_ * 128, (n_ + 1) * 128
                bi = nbank()
                for kk in range(8):
                    sch.op("pe", lambda bi=bi, kk=kk, n0=n0, n1=n1: nc.tensor.matmul(
                        banks[bi][:, :], lhsT=k.wo[:, kk, n0:n1], rhs=merged[:, kk, :], start=(kk == 0), stop=(kk == 7)),
                        reads=[k.t_wF, t_merged], writes=[t_banks[bi]])
                sch.op("dve", lambda bi=bi, n_=n_: nc.vector.scalar_tensor_tensor(
                    out=x1T[:, n_, :], in0=banks[bi][:, :], scalar=k.modv[:, 16 + n_:17 + n_], in1=x_t[:, n_, :],
                    op0=ALU.mult, op1=ALU.add),
                    reads=[t_banks[bi], t_x, k.t_modv], writes=[t_x1T])
            sch.op("act", lambda: nc.scalar.activation(out=h2T[:, :, :], in_=x1T[:, :, :], func=AF.Square),
                   reads=[t_x1T], writes=[t_h2T])
            bi = nbank()
            for kk in range(8):
                sch.op("pe", lambda bi=bi, kk=kk: nc.tensor.matmul(
                    banks[bi][:, :], lhsT=k.ones_f, rhs=h2T[:, kk, :], start=(kk == 0), stop=(kk == 7)),
                    reads=[t_h2T, k.t_cf], writes=[t_banks[bi]])
            sch.op("act", lambda bi=bi: nc.scalar.activation(out=rs[:, :], in_=banks[bi][:, :], func=AF.Sqrt,
                                                              scale=1.0 / D, bias=EPS),
                   reads=[t_banks[bi]], writes=[t_rs])
            sch.op("dve", lambda: nc.vector.reciprocal(out=rs[:, :], in_=rs[:, :]), reads=[t_rs], writes=[t_rs])
            for kk in range(8):
                ia = cnt["tA"] % 2
                cnt["tA"] += 1
                sch.op("dve", lambda kk=kk, ia=ia: nc.vector.scalar_tensor_tensor(
                    out=tA[ia][:, :], in0=x1T[:, kk, :], scalar=k.modv[:, 56 + kk:57 + kk], in1=rs[:, :],
                    op0=ALU.mult, op1=ALU.mult),
                    reads=[t_x1T, t_rs, k.t_modv], writes=[t_tA[ia]])
                sch.op("act", lambda kk=kk, ia=ia: nc.scalar.activation(
                    out=h2T[:, kk, :], in_=tA[ia][:, :], func=AF.Identity, bias=k.modv[:, 24 + kk:25 + kk], scale=1.0),
                    reads=[t_tA[ia], k.t_modv], writes=[t_h2T])
                sch.op("pool", lambda kk=kk: nc.gpsimd.tensor_copy(out=h2Tb[:, kk, :], in_=h2T[:, kk, :]),
                       reads=[t_h2T], writes=[t_h2Tb])
            for jc in range(2):
                j0, j1 = jc * 128, (jc + 1) * 128
                bg, bu = nbank(), nbank()
                for (bi, w) in ((bg, k.wsg), (bu, k.wsu)):
                    for kk in range(8):
                        sch.op("pe", lambda bi=bi, w=w, kk=kk, j0=j0, j1=j1: nc.tensor.matmul(
                            banks[bi][:, :], lhsT=w[:, kk, j0:j1], rhs=h2Tb[:, kk, :], start=(kk == 0), stop=(kk == 7)),
                            reads=[k.t_wF, t_h2Tb], writes=[t_banks[bi]])
                ia = cnt["tA"] % 2
                cnt["tA"] += 1
                sch.op("act", lambda bg=bg, ia=ia: nc.scalar.activation(out=tA[ia][:, :], in_=banks[bg][:, :], func=AF.Silu),
                       reads=[t_banks[bg]], writes=[t_tA[ia]])
                sch.op("dve", lambda bu=bu, ia=ia, jc=jc: nc.vector.tensor_tensor(
                    out=hid[:, jc, :], in0=banks[bu][:, :], in1=tA[ia][:, :], op=ALU.mult),
                    reads=[t_banks[bu], t_tA[ia]], writes=[t_hid])
            for s in range(4):
                gt = Tt * 4 + s
                s0, s1 = s * 128, (s + 1) * 128
                si = cnt["s"] % 2
                cnt["s"] += 1
                for hf in range(2):
                    bi = nbank()
                    for q in range(4):
                        kk = hf * 4 + q
                        sch.op("pe", lambda bi=bi, q=q, kk=kk, s0=s0, s1=s1: nc.tensor.transpose(
                            out=banks[bi][:, q * 128:(q + 1) * 128], in_=x1T[:, kk, s0:s1], identity=k.ident_f),
                            reads=[t_x1T, k.t_cf], writes=[t_banks[bi]])
                    sch.op("act", lambda bi=bi, si=si, hf=hf: nc.scalar.copy(
                        out=x1tok[si][:, hf * 512:(hf + 1) * 512], in_=banks[bi][:, :]),
                        reads=[t_banks[bi]], writes=[t_x1tok[si]])
                for kk in range(8):
                    sch.op("pe", lambda kk=kk, s0=s0, s1=s1: nc.tensor.transpose(
                        out=pH[:, kk * 128:(kk + 1) * 128], in_=h2Tb[:, kk, s0:s1], identity=k.ident_b),
                        reads=[t_h2Tb, k.t_cb], writes=[t_pH])
                sch.op("dve", lambda si=si: nc.vector.tensor_copy(out=h2tok[si][:, :], in_=pH[:, :]),
                       reads=[t_pH], writes=[t_h2tok[si]])
                bl = nbank()
                for kk in range(8):
                    sch.op("pe", lambda bl=bl, kk=kk, s0=s0, s1=s1: nc.tensor.matmul(
                        banks[bl][:, 0:NE], lhsT=h2T[:, kk, s0:s1], rhs=k.wr[:, kk, :], start=(kk == 0), stop=(kk == 7)),
                        reads=[t_h2T, k.t_wF], writes=[t_banks[bl]])
                V = nc.vector
                while pending:
                    pending.pop(0)()
                for hf in range(2):
                    bi = nbank()
                    for jc in range(2):
                        sch.op("pe", lambda bi=bi, jc=jc, hf=hf, s0=s0, s1=s1: nc.tensor.matmul(
                            banks[bi][:, :], lhsT=hid[:, jc, s0:s1], rhs=k.wsd[:, jc, hf * 512:(hf + 1) * 512],
                            start=(jc == 0), stop=(jc == 1)),
                            reads=[t_hid, k.t_wF], writes=[t_banks[bi]])
                    ib = cnt["tB"] % 2
                    cnt["tB"] += 1
                    sch.op("dve", lambda bi=bi, ib=ib, hf=hf: V.tensor_tensor(
                        out=tB[ib][:, :], in0=banks[bi][:, :], in1=k.gfbc[:, hf * 512:(hf + 1) * 512], op=ALU.mult),
                        reads=[t_banks[bi], k.t_gfbc], writes=[t_tB[ib]])
                    sch.op("pool", lambda ib=ib, si=si, hf=hf: nc.gpsimd.tensor_tensor(
                        out=xs[si][:, hf * 512:(hf + 1) * 512], in0=tB[ib][:, :], in1=x1tok[si][:, hf * 512:(hf + 1) * 512],
                        op=ALU.add),
                        reads=[t_tB[ib], t_x1tok[si]], writes=[t_xs[si]])
                sch.op("sp", lambda gt=gt, si=si: nc.sync.dma_start(out=k.XS[gt * 128:(gt + 1) * 128, :], in_=xs[si][:, :]),
                       reads=[t_xs[si]], dma=True)
                sch.op("act", lambda bl=bl: nc.scalar.activation(out=sc[:, :], in_=banks[bl][:, 0:NE], func=AF.Sigmoid),
                       reads=[t_banks[bl]], writes=[t_sc])
                sch.op("dve", lambda: V.tensor_tensor(out=ch[:, :], in0=sc[:, :], in1=k.rbias[:, :], op=ALU.add),
                       reads=[t_sc, k.t_wF], writes=[t_ch])
                for g8 in range(8):
                    sch.op("dve", lambda g8=g8: V.max(out=m8[:, g8, :], in_=ch[:, g8 * 32:(g8 + 1) * 32]),
                           reads=[t_ch], writes=[t_m8])
                sch.op("dve", lambda: V.tensor_tensor(out=sm[:, 0:8], in0=m8[:, :, 0], in1=m8[:, :, 1], op=ALU.add),
                       reads=[t_m8], writes=[t_sm])
                sch.op("dve", lambda: V.max(out=sm[:, 8:16], in_=sm[:, 0:8]), reads=[t_sm], writes=[t_sm])
                sch.op("dve", lambda: V.tensor_scalar(out=sm[:, 16:24], in0=sm[:, 0:8], scalar1=sm[:, 11:12], scalar2=None,
                                                      op0=ALU.is_ge), reads=[t_sm], writes=[t_sm])
                sch.op("dve", lambda: V.tensor_scalar(out=sm[:, 24:32], in0=sm[:, 16:24], scalar1=1e9, scalar2=-1e9,
                                                      op0=ALU.mult, op1=ALU.add), reads=[t_sm], writes=[t_sm])
                for g8 in range(8):
                    sch.op("dve", lambda g8=g8: V.tensor_scalar(
                        out=chm[:, g8 * 32:(g8 + 1) * 32], in0=ch[:, g8 * 32:(g8 + 1) * 32],
                        scalar1=sm[:, 16 + g8:17 + g8], scalar2=sm[:, 24 + g8:25 + g8], op0=ALU.mult, op1=ALU.add),
                        reads=[t_ch, t_sm], writes=[t_chm])
                sch.op("dve", lambda: V.max(out=sm[:, 32:40], in_=chm[:, :]), reads=[t_chm], writes=[t_sm])
                sch.op("dve", lambda: V.tensor_scalar(out=sel[:, :], in0=chm[:, :], scalar1=sm[:, 39:40], scalar2=None,
                                                      op0=ALU.is_ge), reads=[t_chm, t_sm], writes=[t_sel])
                sch.op("act", lambda: nc.scalar.copy(out=selb[:, :], in_=sel[:, :]), reads=[t_sel], writes=[t_selb])
                sch.op("dve", lambda: V.scalar_tensor_tensor(out=gsel[:, :], in0=sc[:, :], scalar=1.0, in1=sel[:, :],
                                                             op0=ALU.mult, op1=ALU.mult, accum_out=sm[:, 48:49]),
                       reads=[t_sc, t_sel], writes=[t_gsel, t_sm])
                sch.op("dve", lambda: V.reciprocal(out=sm[:, 49:50], in_=sm[:, 48:49]), reads=[t_sm], writes=[t_sm])
                sch.op("dve", lambda: V.tensor_scalar(out=gsel[:, :], in0=gsel[:, :], scalar1=sm[:, 49:50], scalar2=2.5,
                                                      op0=ALU.mult, op1=ALU.mult), reads=[t_gsel, t_sm], writes=[t_gsel])
                sch.op("dve", lambda: V.tensor_tensor(out=sval[:, :], in0=sel[:, :], in1=k.eplus1, op=ALU.mult),
                       reads=[t_sel, k.t_cg], writes=[t_sval])
                sch.op("dve", lambda: V.max(out=sm[:, 40:48], in_=sval[:, :]), reads=[t_sval], writes=[t_sm])
                for j in range(8):
                    sch.op("dve", lambda gt=gt, j=j: V.scalar_tensor_tensor(
                        out=junk[:, :], in0=sval[:, :], scalar=sm[:, 40 + j:41 + j], in1=gsel[:, :],
                        op0=ALU.is_equal, op1=ALU.mult, accum_out=k.gate_all[:, gt, j:j + 1]),
                        reads=[t_sval, t_sm, t_gsel], writes=[t_junk, k.t_gate])
                def count_ops():
                    br2 = nbank()
                    sch.op("pe", lambda br2=br2: nc.tensor.matmul(banks[br2][:, 0:NE], lhsT=k.cb[:, 896:1024], rhs=selb[:, :],
                                                                   start=True, stop=True),
                           reads=[t_selb, k.t_cb], writes=[t_banks[br2]])
                    sch.op("dve", lambda br2=br2: nc.vector.tensor_tensor(out=k.base[:, :], in0=banks[br2][:, 0:NE], in1=k.base[:, :],
                                                                          op=ALU.add),
                           reads=[t_banks[br2], k.t_base], writes=[k.t_base])
                pending.append(count_ops)
                sch.op("sp", lambda gt=gt: nc.sync.dma_start(out=k.SELB[gt, :, :], in_=selb[:, :]), reads=[t_selb], dma=True)
                sch.op("sp", lambda gt=gt, si=si: nc.sync.dma_start(out=k.H2TOK[gt * 128:(gt + 1) * 128, :], in_=h2tok[si][:, :]),
                       reads=[t_h2tok[si]], dma=True)
        while pending:
            pending.pop(0)()
        sch.flush("F1")


def phase_F2(k):
    nc, sch = k.nc, k.sch
    with ExitStack() as st:
        sb, ps = k.sb, k.ps
        ntl = sb(st, "ntl", [128, NE], F32)
        ca = sb(st, "ca", [128, NE], F32)
        cbb = sb(st, "cbb", [128, NE], F32)
        tecol = sb(st, "tecol", [128, 2], F32)
        ind = [sb(st, f"ind{i}", [128, NBLK], BF16) for i in range(2)]
        idxf = sb(st, "idxf", [128, NBLK], F32)
        jk = sb(st, "jk2", [128, 128], F32)
        pb = ps(st, "pbexp", [128, NBLK])
        T = Tok
        t_ntl, t_ca, t_cbb, t_tecol, t_idxf, t_jk, t_pb = (T() for _ in range(7))
        t_ind = [T(), T()]
        V = nc.vector
        sch.op("dve", lambda: V.tensor_scalar(out=ntl[:, :], in0=k.base[:, :], scalar1=0.0, scalar2=None, op0=ALU.is_gt),
               reads=[k.t_base], writes=[t_ntl])
        for m in range(1, S // BS):
            sch.op("dve", lambda m=m: V.scalar_tensor_tensor(out=ntl[:, :], in0=k.base[:, :], scalar=float(BS) * m, in1=ntl[:, :],
                                                             op0=ALU.is_gt, op1=ALU.add),
                   reads=[k.t_base, t_ntl], writes=[t_ntl])
        src, tsrc = ntl, t_ntl
        bufs = [(ca, t_ca), (cbb, t_cbb)]
        for si, sh in enumerate((1, 2, 4, 8, 16, 32, 64, 128)):
            dst, tdst = bufs[si % 2]
            sch.op("dve", lambda src=src, dst=dst, sh=sh: V.tensor_tensor(
                out=dst[:, sh:NE], in0=src[:, sh:NE], in1=src[:, 0:NE - sh], op=ALU.add),
                reads=[tsrc], writes=[tdst])
            sch.op("dve", lambda src=src, dst=dst, sh=sh: V.tensor_copy(out=dst[:, 0:sh], in_=src[:, 0:sh]),
                   reads=[tsrc], writes=[tdst])
            src, tsrc = dst, tdst
        tend, t_tend = src, tsrc
        sch.op("dve", lambda: V.tensor_tensor(out=k.base[:, :], in0=tend[:, :], in1=ntl[:, :], op=ALU.subtract),
               reads=[t_tend, t_ntl], writes=[k.t_base])
        sch.op("dve", lambda: V.tensor_scalar(out=k.base[:, :], in0=k.base[:, :], scalar1=float(BS), scalar2=1.0,
                                              op0=ALU.mult, op1=ALU.add), reads=[k.t_base], writes=[k.t_base])
        for q in range(2):
            sch.op("dve", lambda q=q: V.scalar_tensor_tensor(
                out=jk[:, :], in0=tend[:, q * 128:(q + 1) * 128], scalar=1.0, in1=k.ident_f, op0=ALU.mult, op1=ALU.mult,
                accum_out=tecol[:, q:q + 1]), reads=[t_tend, k.t_cf], writes=[t_jk, t_tecol])
        for q in range(2):
            sch.op("dve", lambda q=q: V.tensor_scalar(out=ind[q][:, :], in0=k.jrow[:, 0:NBLK], scalar1=tecol[:, q:q + 1], scalar2=None,
                                                      op0=ALU.is_ge), reads=[t_tecol, k.t_cg], writes=[t_ind[q]])
            sch.op("pe", lambda q=q: nc.tensor.matmul(pb[:, :], lhsT=k.cb[:, 896:1024], rhs=ind[q][:, :],
                                                       start=(q == 0), stop=(q == 1)),
                   reads=[t_ind[q], k.t_cb], writes=[t_pb])
        sch.op("dve", lambda: V.tensor_scalar(out=idxf[:, :], in0=pb[:, :], scalar1=128.0, scalar2=None,
                                              op0=ALU.mult), reads=[t_pb], writes=[t_idxf])
        sch.op("dve", lambda: V.tensor_scalar(out=idxf[:, :], in0=idxf[:, :], scalar1=k.pcol, scalar2=None,
                                              op0=ALU.add), reads=[t_idxf, k.t_cg], writes=[t_idxf])
        sch.op("dve", lambda: V.tensor_copy(out=k.idx_all[:, :], in_=idxf[:, :]), reads=[t_idxf], writes=[k.t_idx])
        if "IDX" in k.debug:
            sch.op("sp", lambda: nc.sync.dma_start(out=k.IDX[:, :], in_=k.idx_all[:, :]), reads=[k.t_idx], dma=True)
        sch.flush("F2")


def phase_F3(k):
    nc, sch = k.nc, k.sch
    with ExitStack() as st:
        sb, ps = k.sb, k.ps
        selb = [sb(st, f"selb3{i}", [128, NE], BF16) for i in range(2)]
        h2t = [sb(st, f"h2t3{i}", [128, D], BF16) for i in range(3)]
        sval = [sb(st, f"sval3{i}", [128, NE], F32) for i in range(2)]
        s8 = [sb(st, f"s83{i}", [128, 8], F32) for i in range(2)]
        pr1 = [ps(st, f"pr1{i}", [128, NE]) for i in range(2)]
        pr2 = [ps(st, f"pr2{i}", [128, NE]) for i in range(2)]
        T = Tok
        t_selb, t_sval, t_s8, t_pr1, t_pr2 = ([T(), T()] for _ in range(5))
        t_h2t = [T(), T(), T()]
        V = nc.vector
        for gt in range(NT):
            b = gt % 2
            hb = gt % 3
            sch.op("sp", lambda gt=gt, b=b: nc.sync.dma_start(out=selb[b][:, :], in_=k.SELB[gt, :, :]), writes=[t_selb[b]], dma=True)
            sch.op("sp", lambda gt=gt, hb=hb: nc.sync.dma_start(out=h2t[hb][:, :], in_=k.H2TOK[gt * 128:(gt + 1) * 128, :]),
                   writes=[t_h2t[hb]], dma=True)
            sch.op("pe", lambda b=b: nc.tensor.matmul(pr1[b][:, :], lhsT=k.Ustrict, rhs=selb[b][:, :], start=True, stop=True),
                   reads=[t_selb[b], k.t_cb], writes=[t_pr1[b]])
            sch.op("pe", lambda b=b: nc.tensor.matmul(pr2[b][:, :], lhsT=k.cb[:, 896:1024], rhs=selb[b][:, :], start=True, stop=True),
                   reads=[t_selb[b], k.t_cb], writes=[t_pr2[b]])
            sch.op("dve", lambda b=b: V.tensor_tensor(out=sval[b][:, :], in0=pr1[b][:, :], in1=k.base[:, :], op=ALU.add),
                   reads=[t_pr1[b], k.t_base], writes=[t_sval[b]])
            sch.op("dve", lambda b=b: V.tensor_tensor(out=sval[b][:, :], in0=sval[b][:, :], in1=selb[b][:, :], op=ALU.mult),
                   reads=[t_sval[b], t_selb[b]], writes=[t_sval[b]])
            sch.op("dve", lambda b=b: V.tensor_tensor(out=k.base[:, :], in0=pr2[b][:, :], in1=k.base[:, :], op=ALU.add),
                   reads=[t_pr2[b], k.t_base], writes=[k.t_base])
            sch.op("dve", lambda b=b: V.max(out=s8[b][:, :], in_=sval[b][:, :]), reads=[t_sval[b]], writes=[t_s8[b]])
            sch.op("dve", lambda gt=gt, b=b: V.tensor_scalar(out=k.slot_all[:, gt, :], in0=s8[b][:, :], scalar1=-1.0, scalar2=None,
                                                             op0=ALU.add), reads=[t_s8[b]], writes=[k.t_slot])
            for j in range(8):
                sch.op("pool", lambda gt=gt, j=j, hb=hb: nc.gpsimd.indirect_dma_start(
                    out=k.XG[:, :], out_offset=bass.IndirectOffsetOnAxis(ap=k.slot_all[:, gt, j:j + 1], axis=0),
                    in_=h2t[hb][:, :], in_offset=None),
                    reads=[k.t_slot, t_h2t[hb]], dma=True)
        if "SLOT" in k.debug:
            sch.op("sp", lambda: nc.sync.dma_start(out=k.SLOT[:, :, :], in_=k.slot_all[:, :, :]), reads=[k.t_slot], dma=True)
            sch.op("sp", lambda: nc.sync.dma_start(out=k.GATE[:, :, :], in_=k.gate_all[:, :, :]), reads=[k.t_gate], dma=True)
        sch.flush("F3")


def phase_G(k):
    nc, sch = k.nc, k.sch
    with ExitStack() as st:
        sb, ps = k.sb, k.ps
        wf = [sb(st, f"wfa{i}", [128, 6144], F32) for i in range(4)]
        wgb = [sb(st, f"wgb{i}", [128, 8, 256], BF16) for i in range(2)]
        wub = [sb(st, f"wub{i}", [128, 8, 256], BF16) for i in range(2)]
        wdb = [sb(st, f"wdb{i}", [128, 2, D], BF16) for i in range(2)]
        xg = [sb(st, f"xg{i}", [128, TPB, D], BF16) for i in range(4)]
        xgT = [sb(st, f"xgT{i}", [128, 8, BS], BF16) for i in range(2)]
        sgt = [sb(st, f"sgt{i}", [128, 2, BS], F32) for i in range(2)]
        hidT = [sb(st, f"hidT{i}", [128, 2, BS], BF16) for i in range(2)]
        ysb = [sb(st, f"ysb{i}", [128, D], BF16) for i in range(3)]
        pT = [ps(st, f"gpT{i}", [128, D], BF16) for i in range(2)]
        pG = [ps(st, f"gpG{i}", [128, 2, BS]) for i in range(2)]
        pU = [ps(st, f"gpU{i}", [128, 2, BS]) for i in range(2)]
        pY = [ps(st, f"gpY{i}", [128, 512]) for i in range(2)]
        T = Tok
        t_wf = [T(), T(), T(), T()]
        t_wgb, t_wub, t_wdb = ([T(), T()] for _ in range(3))
        t_xg = [T() for _ in range(4)]
        t_xgT, t_sgt, t_hidT = [T(), T()], [T(), T()], [T(), T()]
        t_ysb = [T() for _ in range(3)]
        t_pT = [T(), T()]
        t_pG, t_pU = [T(), T()], [T(), T()]
        t_pY = [T() for _ in range(2)]
        cnt = {"py": 0}
        NROW = NE * 128
        regs = {}

        def gather(j, q):
            f = q % 4
            off = bass.IndirectOffsetOnAxis(ap=k.idx_all[:, j:j + 1], axis=0)

            def fn(f=f, off=off):
                if "r" not in regs:
                    regs["r"] = nc.gpsimd.to_reg(NROW - 1)
                return nc.gpsimd.indirect_dma_start(out=wf[f][:, :], out_offset=None, in_=k.w_all[:, :], in_offset=off,
                                                    bounds_check=regs["r"], oob_is_err=False)
            sch.op("pool", fn, reads=[k.t_idx], writes=[t_wf[f]], dma=True)

        def xload(j, q):
            xi = q % 4
            sch.op("sp", lambda xi=xi, row0=j * BS: nc.sync.dma_start(
                out=xg[xi][:, :, :], in_=k.XG[row0:row0 + BS, :].rearrange("(t p) d -> p t d", p=128)),
                writes=[t_xg[xi]], dma=True)

        def cast(j, which):
            f, b = j % 4, j % 2
            if which == "g":
                sch.op("dve", lambda: nc.vector.tensor_copy(
                    out=wgb[b][:, :, :], in_=wf[f][:, 0:2048].rearrange("p (a c) -> p a c", a=8)),
                    reads=[t_wf[f]], writes=[t_wgb[b]])
            elif which == "u":
                sch.op("act", lambda: nc.scalar.copy(
                    out=wub[b][:, :, :], in_=wf[f][:, 2048:4096].rearrange("p (a c) -> p a c", a=8)),
                    reads=[t_wf[f]], writes=[t_wub[b]])
            elif which == "d0":
                sch.op("dve", lambda: nc.vector.tensor_copy(out=wdb[b][:, 0, :], in_=wf[f][:, 4096:5120]),
                       reads=[t_wf[f]], writes=[t_wdb[b]])
            else:
                sch.op("act", lambda: nc.scalar.copy(out=wdb[b][:, 1, :], in_=wf[f][:, 5120:6144]),
                       reads=[t_wf[f]], writes=[t_wdb[b]])

        tcnt = {"n": 0, "y": 0}

        def t_stage(j):
            xi, b = j % 4, j % 2
            for t in range(TPB):
                pi = tcnt["n"] % 2
                tcnt["n"] += 1
                for kk in range(8):
                    sch.op("pe", lambda kk=kk, t=t, pi=pi: nc.tensor.transpose(
                        out=pT[pi][:, kk * 128:(kk + 1) * 128], in_=xg[xi][:, t, kk * 128:(kk + 1) * 128], identity=k.ident_b),
                        reads=[t_xg[xi], k.t_cb], writes=[t_pT[pi]])
                srcv = pT[pi][:, :].rearrange("p (kk s) -> p kk s", kk=8)
                dstv = xgT[b][:, :, t * 128:(t + 1) * 128]
                if pi == 0:
                    sch.op("act", lambda srcv=srcv, dstv=dstv: nc.scalar.copy(out=dstv, in_=srcv), reads=[t_pT[pi]], writes=[t_xgT[b]])
                else:
                    sch.op("dve", lambda srcv=srcv, dstv=dstv: nc.vector.tensor_copy(out=dstv, in_=srcv), reads=[t_pT[pi]], writes=[t_xgT[b]])

        def gu_stage(j):
            b = j % 2
            for (pp, tp, wb_, twb) in ((pG[b], t_pG[b], wgb, t_wgb), (pU[b], t_pU[b], wub, t_wub)):
                for jc in range(2):
                    for kk in range(8):
                        sch.op("pe", lambda pp=pp, wb_=wb_, jc=jc, kk=kk: nc.tensor.matmul(
                            pp[:, jc, :], lhsT=wb_[b][:, kk, jc * 128:(jc + 1) * 128], rhs=xgT[b][:, kk, :],
                            start=(kk == 0), stop=(kk == 7)),
                            reads=[twb[b], t_xgT[b]], writes=[tp])
            sch.op("act", lambda: nc.scalar.activation(out=sgt[b][:, :, :], in_=pG[b][:, :, :], func=AF.Silu),
                   reads=[t_pG[b]], writes=[t_sgt[b]])
            sch.op("dve", lambda: nc.vector.tensor_tensor(
                out=hidT[b][:, :, :], in0=pU[b][:, :, :], in1=sgt[b][:, :, :], op=ALU.mult),
                reads=[t_pU[b], t_sgt[b]], writes=[t_hidT[b]])

        def d_stage(q, j):
            b = q % 2
            for t in range(TPB):
                yi = tcnt["y"] % 3
                tcnt["y"] += 1
                for hf in range(2):
                    for jc in range(2):
                        sch.op("pe", lambda jc=jc, hf=hf, t=t: nc.tensor.matmul(
                            pY[hf][:, :], lhsT=hidT[b][:, jc, t * 128:(t + 1) * 128], rhs=wdb[b][:, jc, hf * 512:(hf + 1) * 512],
                            start=(jc == 0), stop=(jc == 1)),
                            reads=[t_hidT[b], t_wdb[b]], writes=[t_pY[hf]])
                    if hf == 0:
                        sch.op("act", lambda yi=yi: nc.scalar.copy(out=ysb[yi][:, 0:512], in_=pY[0][:, :]),
                               reads=[t_pY[0]], writes=[t_ysb[yi]])
                    else:
                        sch.op("dve", lambda yi=yi: nc.vector.tensor_copy(out=ysb[yi][:, 512:1024], in_=pY[1][:, :]),
                               reads=[t_pY[1]], writes=[t_ysb[yi]])
                sch.op("sp", lambda yi=yi, row0=j * BS + t * 128: nc.sync.dma_start(out=k.YE[row0:row0 + 128, :], in_=ysb[yi][:, :]),
                       reads=[t_ysb[yi]], dma=True)

        order = []
        lo, hi = 0, NBLK - 1
        while lo <= hi:
            order.append(lo)
            lo += 1
            if lo <= hi:
                order.append(hi)
                hi -= 1
        def at(q):
            return order[q]

        NQ = len(order)
        for q0 in range(3):
            gather(at(q0), q0)
            xload(at(q0), q0)
        for w_ in ("g", "u", "d0", "d1"):
            cast(0, w_)
        cast(1, "g")
        cast(1, "u")
        t_stage(0)
        t_stage(1)
        gu_stage(0)
        cast(1, "d0")
        cast(1, "d1")
        for q in range(NQ):
            if q + 3 < NQ:
                gather(at(q + 3), q + 3)
                xload(at(q + 3), q + 3)
            if q + 2 < NQ:
                t_stage(q + 2)
            if q + 1 < NQ:
                gu_stage(q + 1)
            if q + 2 < NQ:
                cast(q + 2, "g")
                cast(q + 2, "u")
            d_stage(q, at(q))
            if q + 2 < NQ:
                cast(q + 2, "d0")
                cast(q + 2, "d1")
        sch.flush("G")


def phase_H(k):
    nc, sch = k.nc, k.sch
    with ExitStack() as st:
        sb = k.sb
        nf = sb(st, "nf", [128, D], F32)
        xs_t = [sb(st, f"hxs{i}", [128, D], F32) for i in range(2)]
        yk = [sb(st, f"yk{i}", [128, D], BF16) for i in range(16)]
        acc = [sb(st, f"acc{i}", [128, D], F32) for i in range(2)]
        sqj = sb(st, "sqj", [128, D], F32)
        ot = [sb(st, f"ot{i}", [128, D], F32) for i in range(2)]
        stat = sb(st, "stat", [128, 4], F32)
        T = Tok
        t_nf, t_sqj, t_stat = T(), T(), T()
        t_xs, t_acc, t_ot = [T(), T()], [T(), T()], [T(), T()]
        t_yk = [T() for _ in range(16)]
        sch.op("sp", lambda: nc.sync.dma_start(out=nf[:, :], in_=k.nfin_bc[:, :]), writes=[t_nf], dma=True)
        V = nc.vector
        for gt in range(NT):
            b = gt % 2
            sch.op("sp", lambda gt=gt, b=b: nc.sync.dma_start(out=xs_t[b][:, :], in_=k.XS[gt * 128:(gt + 1) * 128, :]),
                   writes=[t_xs[b]], dma=True)
            for j in range(8):
                yi = b * 8 + j
                sch.op("pool", lambda gt=gt, j=j, yi=yi: nc.gpsimd.indirect_dma_start(
                    out=yk[yi][:, :], out_offset=None, in_=k.YE[:, :],
                    in_offset=bass.IndirectOffsetOnAxis(ap=k.slot_all[:, gt, j:j + 1], axis=0)),
                    reads=[k.t_slot], writes=[t_yk[yi]], dma=True)
            for j in range(8):
                yi = b * 8 + j
                if j == 0:
                    sch.op("dve", lambda gt=gt, b=b, yi=yi: V.tensor_scalar(
                        out=acc[b][:, :], in0=yk[yi][:, :], scalar1=k.gate_all[:, gt, 0:1], scalar2=None, op0=ALU.mult),
                        reads=[t_yk[yi], k.t_gate], writes=[t_acc[b]])
                else:
                    sch.op("dve", lambda gt=gt, b=b, yi=yi, j=j: V.scalar_tensor_tensor(
                        out=acc[b][:, :], in0=yk[yi][:, :], scalar=k.gate_all[:, gt, j:j + 1], in1=acc[b][:, :],
                        op0=ALU.mult, op1=ALU.add),
                        reads=[t_yk[yi], k.t_gate, t_acc[b]], writes=[t_acc[b]])
            sch.op("dve", lambda b=b: V.tensor_tensor(out=acc[b][:, :], in0=acc[b][:, :], in1=k.gfbc[:, :], op=ALU.mult),
                   reads=[t_acc[b], k.t_gfbc], writes=[t_acc[b]])
            sch.op("dve", lambda b=b: V.tensor_tensor(out=acc[b][:, :], in0=acc[b][:, :], in1=xs_t[b][:, :], op=ALU.add),
                   reads=[t_acc[b], t_xs[b]], writes=[t_acc[b]])
            sch.op("act", lambda b=b: nc.scalar.activation(out=sqj[:, :], in_=acc[b][:, :], func=AF.Square, accum_out=stat[:, 0:1]),
                   reads=[t_acc[b]], writes=[t_sqj, t_stat])
            sch.op("act", lambda: nc.scalar.activation(out=stat[:, 1:2], in_=stat[:, 0:1], func=AF.Sqrt, scale=1.0 / D, bias=EPS),
                   reads=[t_stat], writes=[t_stat])
            sch.op("dve", lambda: V.reciprocal(out=stat[:, 2:3], in_=stat[:, 1:2]), reads=[t_stat], writes=[t_stat])
            sch.op("dve", lambda b=b: V.scalar_tensor_tensor(
                out=ot[b][:, :], in0=acc[b][:, :], scalar=stat[:, 2:3], in1=nf[:, :], op0=ALU.mult, op1=ALU.mult),
                reads=[t_acc[b], t_stat, t_nf], writes=[t_ot[b]])
            sch.op("sp", lambda gt=gt, b=b: nc.sync.dma_start(out=k.out[gt * 128:(gt + 1) * 128, :], in_=ot[b][:, :]),
                   reads=[t_ot[b]], dma=True)
        sch.flush("H")
```

```python
import math
from contextlib import ExitStack
import numpy as np
import ml_dtypes
import concourse.bass as bass
import concourse.mybir as mybir
from concourse.bass_utils import run_bass_kernel_spmd

F32 = mybir.dt.float32
BF16 = mybir.dt.bfloat16
I32 = mybir.dt.int32
U32 = mybir.dt.uint32
AF = mybir.ActivationFunctionType
ALU = mybir.AluOpType
AX = mybir.AxisListType

D = 1024
S = 4096
NT = 32
PROJ = 9216
NE = 256
BS = 256
TPB = BS // 128
NBLK = 32768 // BS + 256
NSLOT = NBLK * BS
EPS = 1e-6


class Tok:
    __slots__ = ("w", "r", "name")

    def __init__(self, name=""):
        self.w = None
        self.r = []
        self.name = name


class Op:
    __slots__ = ("eng", "fn", "deps", "dma", "sig", "sem", "val", "idx", "prewait", "ph")

    def __init__(self, eng, fn, dma):
        self.eng = eng
        self.fn = fn
        self.dma = dma
        self.deps = []
        self.sig = False
        self.sem = None
        self.val = 0
        self.prewait = None


ENGS = ("pe", "act", "dve", "pool", "sp")


class Sched:
    def __init__(self, nc, stack):
        self.nc = nc
        self.stack = stack
        self.ops = []
        self.eobj = {"pe": nc.tensor, "act": nc.scalar, "dve": nc.vector,
                     "pool": nc.gpsimd, "sp": nc.sync}
        self.dma_sems = {}
        for q, n in (("sp", 24), ("act", 8), ("pool", 12)):
            self.dma_sems[q] = [[stack.enter_context(nc.semaphore(f"d{q}{i}")), 0, None]
                                for i in range(n)]
        self.dma_rr = {"sp": 0, "act": 0, "pool": 0}
        self.phase_i = 0

    def op(self, eng, fn, reads=(), writes=(), dma=False):
        o = Op(eng, fn, dma)
        o.idx = len(self.ops)
        o.ph = self.phase_i
        deps = set()

        def same(d):
            return (not dma) and (not d.dma) and d.eng == eng

        for t in reads:
            if t.w is not None:
                deps.add(t.w)
        for t in writes:
            if t.w is not None and not same(t.w):
                deps.add(t.w)
            for r in t.r:
                if not same(r):
                    deps.add(r)
        deps.discard(o)
        o.deps = [d for d in deps if d.ph == self.phase_i]
        for t in reads:
            t.r.append(o)
        for t in writes:
            t.w = o
            t.r = []
        self.ops.append(o)
        return o

    def flush(self, name):
        nc = self.nc
        ops = self.ops
        self.ops = []
        if not ops:
            return
        for o in ops:
            for d in o.deps:
                if not d.dma:
                    if d.eng == "pe" and o.eng == "pe" and not o.dma:
                        continue
                    d.sig = True
        with ExitStack() as st:
            esem = {e: st.enter_context(nc.semaphore(f"p{self.phase_i}{e}")) for e in ENGS}
            ecount = {e: 0 for e in ENGS}
            for o in ops:
                if o.dma:
                    pool = self.dma_sems[o.eng]
                    i = self.dma_rr[o.eng]
                    self.dma_rr[o.eng] = (i + 1) % len(pool)
                    ent = pool[i]
                    o.prewait = (ent[0], ent[1]) if ent[1] > 0 else None
                    ent[1] += 16
                    o.sem = ent[0]
                    o.val = ent[1]
                elif o.sig:
                    ecount[o.eng] += 1
                    o.sem = esem[o.eng]
                    o.val = ecount[o.eng]
            per = {e: [o for o in ops if o.eng == e] for e in ENGS}
            with nc.Block() as block:
                def run(e, eng):
                    known = {}

                    def wait(sem, val):
                        k = id(sem)
                        if known.get(k, 0) >= val:
                            return
                        known[k] = val
                        eng.wait_ge(sem, val)

                    for o in per[e]:
                        for d in o.deps:
                            if d.dma:
                                wait(d.sem, d.val)
                            else:
                                if d.eng == "pe" and e == "pe" and not o.dma:
                                    continue
                                wait(d.sem, d.val)
                        if o.dma and o.prewait is not None:
                            wait(*o.prewait)
                        inst = o.fn()
                        if o.dma:
                            inst.then_inc(o.sem, 16)
                        elif o.sig:
                            inst.then_inc(o.sem, 1)
                    if e in self.dma_sems:
                        for ent in self.dma_sems[e]:
                            if ent[1] > 0:
                                wait(ent[0], ent[1])

                if per["pe"]:
                    @block.tensor
                    def _(eng):
                        run("pe", eng)
                if per["act"]:
                    @block.scalar
                    def _(eng):
                        run("act", eng)
                if per["dve"]:
                    @block.vector
                    def _(eng):
                        run("dve", eng)
                if per["pool"]:
                    @block.gpsimd
                    def _(eng):
                        run("pool", eng)
                if per["sp"]:
                    @block.sync
                    def _(eng):
                        run("sp", eng)
        self.phase_i += 1


def _bf16(a):
    return np.ascontiguousarray(a).astype(ml_dtypes.bfloat16)


class K:
    pass


def build_nc(stop_after="H", debug=()):
    nc = bass.Bass("TRN2", target_bir_lowering=False)
    k = K()
    k.nc = nc
    k.debug = set(debug)

    def din(name, shape, dt=F32):
        return nc.dram_tensor(name, list(shape), dt, kind="ExternalInput").ap()

    def dscr(name, shape, dt):
        kind = "ExternalOutput" if name in k.debug else "Internal"
        return nc.dram_tensor(name, list(shape), dt, kind=kind).ap()

    k.xT = din("xT", [8, 128, S])
    k.c_l = din("c_l", [128, 8])
    k.w_ada = din("w_ada", [D, 6 * D])
    k.b_ada_l = din("b_ada_l", [128, 48])
    k.b_gf_bc = din("b_gf_bc", [128, D])
    k.nmix_l = din("nmix_l", [128, 8])
    k.nffn_l = din("nffn_l", [128, 8])
    k.w_in = din("w_in", [D, PROJ])
    k.rdecay_bc = din("rdecay_bc", [128, 8])
    k.t5b = din("t5b", [8, 128, 6 * 384])
    k.w_ret_up = din("w_ret_up", [D, D])
    k.w_att_up = din("w_att_up", [D, D])
    k.w_o = din("w_o", [D, D])
    k.w_router = din("w_router", [D, NE])
    k.rbias_bc = din("rbias_bc", [128, NE])
    if stop_after >= "G":
        k.w_all = din("w_all", [NE * 128, 6144])
    k.ws_gate = din("ws_gate", [D, 256])
    k.ws_up = din("ws_up", [D, 256])
    k.ws_down = din("ws_down", [256, D])
    k.nfin_bc = din("nfin_bc", [128, D])
    k.cosT = din("cosT", [128, S])
    k.sinT = din("sinT", [128, S])
    k.cst_f32 = din("cst_f32", [128, 1280])
    k.cst_bf = din("cst_bf", [128, 1024], BF16)
    k.cst_g = din("cst_g", [128, 1024])
    k.out = nc.dram_tensor("out", [S, D], F32, kind="ExternalOutput").ap()

    for nm in ("QF", "QB", "KF", "KB", "SG", "AQ", "AK", "GR", "GA", "ORET", "OATT"):
        setattr(k, nm, dscr(nm, [8, 128, S], BF16))
    k.RV = dscr("RV", [S, D], BF16)
    k.AV = dscr("AV", [S, D], BF16)
    k.XS = dscr("XS", [S, D], F32)
    if stop_after >= "F":
        k.XG = dscr("XG", [NSLOT, D], BF16)
        k.YE = dscr("YE", [NSLOT, D], BF16)
        k.SELB = dscr("SELB", [NT, 128, NE], BF16)
        k.H2TOK = dscr("H2TOK", [S, D], BF16)
    k.HT = dscr("HT", [8, 128, S], BF16) if "HT" in k.debug else None
    k.MOD = dscr("MOD", [128, 64], F32) if "MOD" in k.debug else None
    if "SLOT" in k.debug:
        k.SLOT = dscr("SLOT", [128, NT, 8], I32)
        k.GATE = dscr("GATE", [128, NT, 8], F32)
    if "IDX" in k.debug:
        k.IDX = dscr("IDX", [128, NBLK], I32)

    with ExitStack() as top:
        sch = Sched(nc, top)
        k.sch = sch
        k.top = top

        def sb(st, name, shape, dt):
            return st.enter_context(nc.sbuf_tensor(name, list(shape), dt))

        def ps(st, name, shape, dt=F32):
            return st.enter_context(nc.psum_tensor(name, list(shape), dt))

        k.sb = sb
        k.ps = ps
        k.cf = sb(top, "cf", [128, 1280], F32)
        k.cb = sb(top, "cb", [128, 1024], BF16)
        k.modv = sb(top, "modv", [128, 64], F32)
        k.gfbc = sb(top, "gfbc", [128, D], F32)
        k.dec = sb(top, "dec", [128, 24], F32)
        k.slot_all = sb(top, "slot_all", [128, NT, 8], I32)
        k.idx_all = sb(top, "idx_all", [128, NBLK], I32)
        k.t_idx = Tok()
        k.gate_all = sb(top, "gate_all", [128, NT, 8], F32)
        k.t_cf, k.t_cb, k.t_modv, k.t_gfbc, k.t_dec = Tok(), Tok(), Tok(), Tok(), Tok()
        k.t_slot, k.t_gate = Tok(), Tok()
        k.ident_f = k.cf[:, 0:128]
        k.ones_f = k.cf[:, 128:256]
        k.posF = k.cf[:, 256:768]
        k.posB = k.cf[:, 768:1280]
        k.ident_b = k.cb[:, 0:128]
        k.maskF = k.cb[:, 128:256]
        k.maskB = k.cb[:, 256:384]
        k.Ustrict = k.cb[:, 384:512]
        k.attmask = k.cb[:, 512:896]
        k.ones_b = None

        if stop_after == "A":
            phase_A(k)
            return nc
        with ExitStack() as stBC:
            k.hT = sb(stBC, "hT", [128, 8, S], BF16)
            k.t_hT = [Tok() for _ in range(8)]
            with ExitStack() as stAB:
                phase_A(k, stAB, flush=False)
                phase_B(k)
            if stop_after != "B":
                phase_C(k)
        if stop_after in ("B", "C"):
            return nc
        if "skipD" not in k.debug:
            phase_D(k)
        if stop_after == "D":
            return nc
        if "skipE" not in k.debug:
            phase_E(k)
        if stop_after == "E":
            return nc
        with ExitStack() as stF:
            phase_F0(k, stF)
            phase_F1(k)
            phase_F2(k)
            phase_F3(k)
        if stop_after == "F":
            return nc
        phase_G(k)
        phase_H(k)
    return nc


def phase_A(k, st_outer=None, flush=True):
    nc, sch = k.nc, k.sch
    with ExitStack() as st_own:
        st = st_outer if st_outer is not None else st_own
        sb, ps = k.sb, k.ps
        cond = sb(st, "cond", [128, 8], F32)
        cl = sb(st, "cl", [128, 8], F32)
        condbc = sb(st, "condbc", [128, 8, 128], F32)
        bl = sb(st, "bl", [128, 48], F32)
        nm = sb(st, "nm", [128, 16], F32)
        rd = sb(st, "rd", [128, 8], F32)
        bgf = sb(st, "bgf", [128, D], F32)
        wa = [sb(st, f"wa{i}", [128, 8, 512], F32) for i in range(2)]
        pmod = ps(st, "pmod", [128, 512])
        pgf = [ps(st, f"pgf{i}", [128, 512]) for i in range(2)]
        t_cond, t_cl, t_cbc, t_bl, t_nm, t_rd, t_bgf = (Tok() for _ in range(7))
        t_wa = [Tok(), Tok()]
        t_pmod, t_pgf = Tok(), [Tok(), Tok()]

        sch.op("sp", lambda: nc.sync.dma_start(out=k.cf[:, :], in_=k.cst_f32[:, :]), writes=[k.t_cf], dma=True)
        sch.op("sp", lambda: nc.sync.dma_start(out=k.cb[:, :], in_=k.cst_bf[:, :]), writes=[k.t_cb], dma=True)
        sch.op("sp", lambda: nc.sync.dma_start(out=cl[:, :], in_=k.c_l[:, :]), writes=[t_cl], dma=True)
        sch.op("sp", lambda: nc.sync.dma_start(out=bl[:, :], in_=k.b_ada_l[:, :]), writes=[t_bl], dma=True)
        sch.op("sp", lambda: nc.sync.dma_start(out=nm[:, 0:8], in_=k.nmix_l[:, :]), writes=[t_nm], dma=True)
        sch.op("sp", lambda: nc.sync.dma_start(out=nm[:, 8:16], in_=k.nffn_l[:, :]), writes=[t_nm], dma=True)
        sch.op("sp", lambda: nc.sync.dma_start(out=rd[:, :], in_=k.rdecay_bc[:, :]), writes=[t_rd], dma=True)
        sch.op("sp", lambda: nc.sync.dma_start(out=bgf[:, :], in_=k.b_gf_bc[:, :]), writes=[t_bgf], dma=True)
        sch.op("act", lambda: nc.scalar.activation(out=cond[:, :], in_=cl[:, :], func=AF.Silu),
               reads=[t_cl], writes=[t_cond])
        for kk in range(8):
            sch.op("dve", lambda kk=kk: nc.vector.tensor_scalar(
                out=condbc[:, kk, :], in0=k.ones_f, scalar1=cond[:, kk:kk + 1], scalar2=None, op0=ALU.mult),
                reads=[t_cond, k.t_cf], writes=[t_cbc])
        wv = k.w_ada.rearrange("(kk p) n -> p kk n", p=128)
        for s2 in range(12):
            b = s2 % 2
            s, hs = s2 // 2, s2 % 2
            sch.op("sp", lambda s2=s2, b=b: nc.sync.dma_start(out=wa[b][:, :, :], in_=wv[:, :, s2 * 512:(s2 + 1) * 512]),
                   writes=[t_wa[b]], dma=True)
            for j4 in range(4):
                j = hs * 4 + j4
                for kk in range(8):
                    sch.op("pe", lambda s=s, b=b, j=j, j4=j4, kk=kk: nc.tensor.matmul(
                        pmod[:, s * 8 + j:s * 8 + j + 1], lhsT=wa[b][:, kk, j4 * 128:(j4 + 1) * 128],
                        rhs=cond[:, kk:kk + 1], start=(kk == 0), stop=(kk == 7)),
                        reads=[t_wa[b], t_cond], writes=[t_pmod])
            if s == 5:
                h = hs
                for kk in range(8):
                    sch.op("pe", lambda b=b, h=h, kk=kk: nc.tensor.matmul(
                        pgf[h][:, :], lhsT=condbc[:, kk, :], rhs=wa[b][:, kk, :],
                        start=(kk == 0), stop=(kk == 7)),
                        reads=[t_wa[b], t_cbc], writes=[t_pgf[h]])
        sch.op("dve", lambda: nc.vector.tensor_tensor(out=k.modv[:, 0:48], in0=pmod[:, 0:48], in1=bl[:, :], op=ALU.add),
               reads=[t_pmod, t_bl], writes=[k.t_modv])
        for h in range(2):
            sch.op("dve", lambda h=h: nc.vector.tensor_tensor(
                out=k.gfbc[:, h * 512:(h + 1) * 512], in0=pgf[h][:, :], in1=bgf[:, h * 512:(h + 1) * 512], op=ALU.add),
                reads=[t_pgf[h], t_bgf], writes=[k.t_gfbc])
        sch.op("dve", lambda: nc.vector.scalar_tensor_tensor(
            out=k.modv[:, 48:56], in0=k.modv[:, 8:16], scalar=1.0, in1=nm[:, 0:8], op0=ALU.add, op1=ALU.mult),
            reads=[k.t_modv, t_nm], writes=[k.t_modv])
        sch.op("dve", lambda: nc.vector.scalar_tensor_tensor(
            out=k.modv[:, 56:64], in0=k.modv[:, 32:40], scalar=1.0, in1=nm[:, 8:16], op0=ALU.add, op1=ALU.mult),
            reads=[k.t_modv, t_nm], writes=[k.t_modv])
        sch.op("act", lambda: nc.scalar.activation(out=rd[:, :], in_=rd[:, :], func=AF.Exp),
               reads=[t_rd], writes=[t_rd])
        sch.op("act", lambda: nc.scalar.activation(out=k.dec[:, 0:8], in_=rd[:, :], func=AF.Ln, scale=-1.0, bias=1.0),
               reads=[t_rd], writes=[k.t_dec])
        sch.op("act", lambda: nc.scalar.activation(out=k.dec[:, 8:16], in_=k.dec[:, 0:8], func=AF.Exp, scale=128.0),
               reads=[k.t_dec], writes=[k.t_dec])
        sch.op("dve", lambda: nc.vector.tensor_scalar(out=k.dec[:, 16:24], in0=k.dec[:, 0:8], scalar1=-1.0, scalar2=None,
                                                      op0=ALU.mult), reads=[k.t_dec], writes=[k.t_dec])
        if k.MOD is not None:
            sch.op("sp", lambda: nc.sync.dma_start(out=k.MOD[:, :], in_=k.modv[:, :]), reads=[k.t_modv], dma=True)
        if flush:
            sch.flush("A")


def phase_B(k):
    nc, sch = k.nc, k.sch
    with ExitStack() as st:
        sb, ps = k.sb, k.ps
        xt = [sb(st, f"xt{i}", [128, 8, 512], F32) for i in range(2)]
        sq = [sb(st, f"sq{i}", [128, 8, 512], F32) for i in range(2)]
        rs = [sb(st, f"rs{i}", [128, 512], F32) for i in range(2)]
        tmp = [sb(st, f"tmp{i}", [128, 512], F32) for i in range(2)]
        pss = [ps(st, f"pss{i}", [128, 512]) for i in range(2)]
        t_xt, t_sq, t_rs, t_pss = ([Tok(), Tok()] for _ in range(4))
        t_tmp = [Tok(), Tok()]
        xv = k.xT.rearrange("kk p t -> p kk t")
        for t in range(8):
            b = t % 2
            sch.op("sp", lambda t=t, b=b: nc.sync.dma_start(out=xt[b][:, :, :], in_=xv[:, :, t * 512:(t + 1) * 512]),
                   writes=[t_xt[b]], dma=True)
            sch.op("act", lambda b=b: nc.scalar.activation(out=sq[b][:, :, :], in_=xt[b][:, :, :], func=AF.Square),
                   reads=[t_xt[b]], writes=[t_sq[b]])
            for kk in range(8):
                sch.op("pe", lambda b=b, kk=kk: nc.tensor.matmul(
                    pss[b][:, :], lhsT=k.ones_f, rhs=sq[b][:, kk, :], start=(kk == 0), stop=(kk == 7)),
                    reads=[t_sq[b], k.t_cf], writes=[t_pss[b]])
            sch.op("act", lambda b=b: nc.scalar.activation(out=rs[b][:, :], in_=pss[b][:, :], func=AF.Sqrt,
                                                            scale=1.0 / D, bias=EPS),
                   reads=[t_pss[b]], writes=[t_rs[b]])
            sch.op("dve", lambda b=b: nc.vector.reciprocal(out=rs[b][:, :], in_=rs[b][:, :]),
                   reads=[t_rs[b]], writes=[t_rs[b]])
            for kk in range(8):
                tb = kk % 2
                sch.op("dve", lambda b=b, kk=kk, tb=tb: nc.vector.scalar_tensor_tensor(
                    out=tmp[tb][:, :], in0=xt[b][:, kk, :], scalar=k.modv[:, 48 + kk:49 + kk], in1=rs[b][:, :],
                    op0=ALU.mult, op1=ALU.mult),
                    reads=[t_xt[b], t_rs[b], k.t_modv], writes=[t_tmp[tb]])
                sch.op("act", lambda t=t, kk=kk, tb=tb: nc.scalar.activation(
                    out=k.hT[:, kk, t * 512:(t + 1) * 512], in_=tmp[tb][:, :], func=AF.Identity,
                    bias=k.modv[:, kk:kk + 1], scale=1.0),
                    reads=[t_tmp[tb], k.t_modv], writes=[k.t_hT[t]])
        if k.HT is not None:
            for kk in range(8):
                sch.op("sp", lambda kk=kk: nc.sync.dma_start(out=k.HT[kk, :, :], in_=k.hT[:, kk, :]),
                       reads=k.t_hT, dma=True)
        sch.flush("B")


def phase_C(k):
    nc, sch = k.nc, k.sch
    with ExitStack() as st:
        sb, ps = k.sb, k.ps
        wf = [sb(st, f"wf{i}", [128, 4, 512], F32) for i in range(2)]
        wb = [sb(st, f"wb{i}", [128, 8, 512], BF16) for i in range(2)]
        stg = [sb(st, f"stg{i}", [128, 2048], BF16) for i in range(8)]
        cs = [sb(st, f"cs{i}", [128, 512], F32) for i in range(2)]
        sn = [sb(st, f"sn{i}", [128, 512], F32) for i in range(2)]
        tabs = [sb(st, f"tab{i}", [128, 512], F32) for i in range(2)]
        ta = [sb(st, f"ta{i}", [128, 512], F32) for i in range(4)]
        o12 = [sb(st, f"o12{i}", [128, 512], F32) for i in range(2)]
        bank = [ps(st, f"bk{i}", [128, 512]) for i in range(8)]
        t_wf, t_wb = [Tok(), Tok()], [Tok(), Tok()]
        t_stg = [Tok() for _ in range(8)]
        t_cs, t_sn = [Tok(), Tok()], [Tok(), Tok()]
        t_tab = [Tok(), Tok()]
        t_ta = [Tok() for _ in range(4)]
        t_o12 = [Tok(), Tok()]
        t_bank = [Tok() for _ in range(8)]
        wv = k.w_in.rearrange("(kk p) n -> p kk n", p=128)
        cnt = {"bank": 0, "stg": 0, "cs": 0}

        def nbank():
            i = cnt["bank"] % 8
            cnt["bank"] += 1
            return i

        def nstg():
            i = cnt["stg"] % 8
            cnt["stg"] += 1
            return i

        def load_slab(s):
            b = s % 2
            for hlf in range(2):
                sch.op("sp", lambda s=s, hlf=hlf: nc.sync.dma_start(
                    out=wf[hlf][:, :, :], in_=wv[:, hlf * 4:(hlf + 1) * 4, s * 512:(s + 1) * 512]),
                    writes=[t_wf[hlf]], dma=True)
                sch.op("pool", lambda b=b, hlf=hlf: nc.gpsimd.tensor_copy(
                    out=wb[b][:, hlf * 4:(hlf + 1) * 4, :], in_=wf[hlf][:, :, :]),
                    reads=[t_wf[hlf]], writes=[t_wb[b]])

        def mm_feat(b, j, t, bi):
            for kk in range(8):
                sch.op("pe", lambda b=b, j=j, t=t, bi=bi, kk=kk: nc.tensor.matmul(
                    bank[bi][:, :], lhsT=wb[b][:, kk, j * 128:(j + 1) * 128], rhs=k.hT[:, kk, t * 512:(t + 1) * 512],
                    start=(kk == 0), stop=(kk == 7)),
                    reads=[t_wb[b], k.t_hT[t]], writes=[t_bank[bi]])

        load_slab(0)
        for s in range(18):
            b = s % 2
            if s + 1 < 18:
                load_slab(s + 1)
            typ = s // 2
            if typ in (0, 1):
                dstF, dstB = (k.QF, k.QB) if typ == 0 else (k.KF, k.KB)
                for hh in range(2):
                    head = (s % 2) * 2 + hh
                    for v in range(2):
                        col = v * 4 + head
                        if typ == 0:
                            sch.op("act", lambda v=v, col=col: nc.scalar.activation(
                                out=tabs[v][:, :], in_=(k.posF if v == 0 else k.posB), func=AF.Exp,
                                scale=k.dec[:, col:col + 1]),
                                reads=[k.t_dec, k.t_cf], writes=[t_tab[v]])
                        else:
                            sch.op("act", lambda v=v, col=col: nc.scalar.activation(
                                out=tabs[v][:, :], in_=(k.posF if v == 0 else k.posB), func=AF.Exp,
                                scale=k.dec[:, 16 + col:17 + col], bias=math.log(1.0 / 16.0)),
                                reads=[k.t_dec, k.t_cf], writes=[t_tab[v]])
                    for half in range(2):
                        sg = [nstg() for _ in range(4)]
                        for tt in range(4):
                            t = half * 4 + tt
                            ci = cnt["cs"] % 2
                            cnt["cs"] += 1
                            sch.op("sp", lambda t=t, ci=ci: nc.sync.dma_start(
                                out=cs[ci][:, :], in_=k.cosT[:, t * 512:(t + 1) * 512]), writes=[t_cs[ci]], dma=True)
                            sch.op("sp", lambda t=t, ci=ci: nc.sync.dma_start(
                                out=sn[ci][:, :], in_=k.sinT[:, t * 512:(t + 1) * 512]), writes=[t_sn[ci]], dma=True)
                            b1, b2 = nbank(), nbank()
                            mm_feat(b, hh * 2, t, b1)
                            mm_feat(b, hh * 2 + 1, t, b2)
                            TT = nc.vector.tensor_tensor
                            sch.op("dve", lambda b1=b1, ci=ci: TT(out=ta[0][:, :], in0=bank[b1][:, :], in1=cs[ci][:, :], op=ALU.mult),
                                   reads=[t_bank[b1], t_cs[ci]], writes=[t_ta[0]])
                            sch.op("dve", lambda b2=b2, ci=ci: TT(out=ta[1][:, :], in0=bank[b2][:, :], in1=sn[ci][:, :], op=ALU.mult),
                                   reads=[t_bank[b2], t_sn[ci]], writes=[t_ta[1]])
                            sch.op("dve", lambda b1=b1, ci=ci: TT(out=ta[2][:, :], in0=bank[b1][:, :], in1=sn[ci][:, :], op=ALU.mult),
                                   reads=[t_bank[b1], t_sn[ci]], writes=[t_ta[2]])
                            sch.op("dve", lambda b2=b2, ci=ci: TT(out=ta[3][:, :], in0=bank[b2][:, :], in1=cs[ci][:, :], op=ALU.mult),
                                   reads=[t_bank[b2], t_cs[ci]], writes=[t_ta[3]])
                            PT = nc.gpsimd.tensor_tensor
                            sch.op("dve", lambda: TT(out=o12[0][:, :], in0=ta[0][:, :], in1=ta[1][:, :], op=ALU.subtract),
                                   reads=[t_ta[0], t_ta[1]], writes=[t_o12[0]])
                            sch.op("dve", lambda: TT(out=o12[1][:, :], in0=ta[2][:, :], in1=ta[3][:, :], op=ALU.add),
                                   reads=[t_ta[2], t_ta[3]], writes=[t_o12[1]])
                            for v in range(2):
                                for c in range(2):
                                    si = sg[v * 2 + c]
                                    sch.op("pool", lambda v=v, c=c, si=si, tt=tt: PT(
                                        out=stg[si][:, tt * 512:(tt + 1) * 512], in0=o12[c][:, :], in1=tabs[v][:, :],
                                        op=ALU.mult),
                                        reads=[t_o12[c], t_tab[v]], writes=[t_stg[si]])
                        for v in range(2):
                            for c in range(2):
                                si = sg[v * 2 + c]
                                dst = dstF if v == 0 else dstB
                                sch.op("sp", lambda dst=dst, si=si, head=head, c=c, half=half: nc.sync.dma_start(
                                    out=dst[head * 2 + c, :, half * 2048:(half + 1) * 2048], in_=stg[si][:, :]),
                                    reads=[t_stg[si]], dma=True)
            elif typ in (2, 6):
                dst = k.RV if typ == 2 else k.AV
                c0 = (s % 2) * 512
                for t4 in range(8):
                    si = nstg()
                    for i in range(4):
                        t = t4 * 4 + i
                        bi = nbank()
                        for kk in range(8):
                            sch.op("pe", lambda b=b, t=t, bi=bi, kk=kk: nc.tensor.matmul(
                                bank[bi][:, :], lhsT=k.hT[:, kk, t * 128:(t + 1) * 128], rhs=wb[b][:, kk, :],
                                start=(kk == 0), stop=(kk == 7)),
                                reads=[t_wb[b], k.t_hT[t // 4]], writes=[t_bank[bi]])
                        if i % 2 == 0:
                            sch.op("act", lambda si=si, i=i, bi=bi: nc.scalar.copy(
                                out=stg[si][:, i * 512:(i + 1) * 512], in_=bank[bi][:, :]),
                                reads=[t_bank[bi]], writes=[t_stg[si]])
                        else:
                            sch.op("dve", lambda si=si, i=i, bi=bi: nc.vector.tensor_copy(
                                out=stg[si][:, i * 512:(i + 1) * 512], in_=bank[bi][:, :]),
                                reads=[t_bank[bi]], writes=[t_stg[si]])
                    dv = dst[t4 * 512:(t4 + 1) * 512, c0:c0 + 512].rearrange("(i p) c -> p i c", p=128)
                    sch.op("sp", lambda dv=dv, si=si: nc.sync.dma_start(
                        out=dv, in_=stg[si][:, :].rearrange("p (i c) -> p i c", i=4)),
                        reads=[t_stg[si]], dma=True)
            else:
                dst = {3: k.SG, 4: k.AQ, 5: k.AK, 7: k.GR, 8: k.GA}[typ]
                func = {3: AF.Silu, 4: AF.Copy, 5: AF.Copy, 7: AF.Sigmoid, 8: AF.Sigmoid}[typ]
                for j in range(4):
                    chunk = (s % 2) * 4 + j
                    for half in range(2):
                        si = nstg()
                        for tt in range(4):
                            t = half * 4 + tt
                            bi = nbank()
                            mm_feat(b, j, t, bi)
                            if func == AF.Copy and tt % 2 == 1:
                                sch.op("dve", lambda si=si, tt=tt, bi=bi: nc.vector.tensor_copy(
                                    out=stg[si][:, tt * 512:(tt + 1) * 512], in_=bank[bi][:, :]),
                                    reads=[t_bank[bi]], writes=[t_stg[si]])
                            else:
                                sch.op("act", lambda si=si, tt=tt, bi=bi, func=func: nc.scalar.activation(
                                    out=stg[si][:, tt * 512:(tt + 1) * 512], in_=bank[bi][:, :], func=func),
                                    reads=[t_bank[bi]], writes=[t_stg[si]])
                        sch.op("sp", lambda dst=dst, si=si, chunk=chunk, half=half: nc.sync.dma_start(
                            out=dst[chunk, :, half * 2048:(half + 1) * 2048], in_=stg[si][:, :]),
                            reads=[t_stg[si]], dma=True)
        sch.flush("C")


def _t5_buckets():
    out = np.zeros((3, 128, 384), np.int64)
    kk = np.arange(128)[:, None]
    for g, r in enumerate((1, 4, 16)):
        for t in (-1, 0, 1):
            q = np.arange(128)[None, :]
            rel = (t * 128 + kk - q) * r
            n = np.abs(rel)
            large = 8 + (np.log(np.maximum(n, 1).astype(np.float32) / np.float32(8))
                         / np.float32(math.log(1024 / 8)) * np.float32(8)).astype(np.int32)
            large = np.minimum(large, 15)
            bk = np.where(rel > 0, 16, 0) + np.where(n < 8, n, large)
            bk = np.where(np.abs(t * 128 + kk - q) <= 64, bk, 0)
            out[g, :, (t + 1) * 128:(t + 2) * 128] = bk
    return out


def _constants():
    c = {}
    half = 128
    inv_freq = (np.float32(10000.0) ** (-np.arange(half, dtype=np.float32) / np.float32(half))).astype(np.float32)
    ang = (np.arange(S, dtype=np.float32)[:, None] * inv_freq[None, :]).astype(np.float32)
    c["cosT"] = np.ascontiguousarray(np.cos(ang).astype(np.float32).T)
    c["sinT"] = np.ascontiguousarray(np.sin(ang).astype(np.float32).T)
    cf = np.zeros((128, 1280), np.float32)
    cf[:, 0:128] = np.eye(128, dtype=np.float32)
    cf[:, 128:256] = 1.0
    i = np.arange(128, dtype=np.float32)
    cf[:, 256:768] = np.tile(i + 1.0, 4)[None, :]
    cf[:, 768:1280] = np.tile(128.0 - i, 4)[None, :]
    c["cst_f32"] = cf
    cb = np.zeros((128, 1024), np.float32)
    cb[:, 896:1024] = 1.0
    cb[:, 0:128] = np.eye(128)
    jj = np.arange(128)[:, None]
    ii = np.arange(128)[None, :]
    cb[:, 128:256] = (jj <= ii)
    cb[:, 256:384] = (jj > ii)
    cb[:, 384:512] = (jj < ii)
    for t in (-1, 0, 1):
        cb[:, 512 + (t + 1) * 128:512 + (t + 2) * 128] = (np.abs(t * 128 + jj - ii) <= 64)
    c["cst_bf"] = _bf16(cb)
    cg = np.zeros((128, 1024), np.float32)
    cg[:, 0:512] = np.arange(512, dtype=np.float32)[None, :]
    cg[:, 512:768] = (np.arange(NE, dtype=np.float32) + 1.0)[None, :]
    cg[:, 768] = np.arange(128, dtype=np.float32)
    c["cst_g"] = cg
    return c


def _shared_inputs(inp, upto="H"):
    m = dict(_constants())
    f = lambda a: np.ascontiguousarray(np.asarray(a, dtype=np.float32))
    m["w_ada"] = f(inp["w_ada"][0])
    m["b_ada_l"] = f(inp["b_ada"][0].reshape(48, 128).T)
    m["b_gf_bc"] = f(np.tile(np.asarray(inp["b_ada"])[0, 5 * D:6 * D][None, :], (128, 1)))
    m["nmix_l"] = f(inp["norm_mix"][0].reshape(8, 128).T)
    m["nffn_l"] = f(inp["norm_ffn"][0].reshape(8, 128).T)
    m["w_in"] = f(inp["w_in"][0])
    m["rdecay_bc"] = f(np.tile(np.asarray(inp["ret_decay"])[0].reshape(1, 8), (128, 1)))
    bk = _t5_buckets()
    t5 = np.asarray(inp["t5_bias"], dtype=np.float32)
    tb = t5[bk]
    tb = tb.reshape(3, 128, 384, 8, 2).transpose(3, 1, 0, 4, 2)
    m["t5b"] = f(tb.reshape(8, 128, 6 * 384))
    m["w_ret_up"] = f(inp["w_ret_up"][0])
    m["w_att_up"] = f(inp["w_att_up"][0])
    m["w_o"] = f(inp["w_o"][0])
    m["w_router"] = f(inp["w_router"][0])
    m["rbias_bc"] = f(np.tile(np.asarray(inp["router_bias"])[0][None, :], (128, 1)))
    if upto >= "G":
        wa = np.empty((NE, 128, 3, 2048), np.float32)
        wa[:, :, 0, :] = np.asarray(inp["w_gate"][0]).reshape(NE, 8, 128, 256).transpose(0, 2, 1, 3).reshape(NE, 128, 2048)
        wa[:, :, 1, :] = np.asarray(inp["w_up"][0]).reshape(NE, 8, 128, 256).transpose(0, 2, 1, 3).reshape(NE, 128, 2048)
        wa[:, :, 2, :] = np.asarray(inp["w_down"][0]).reshape(NE, 2, 128, D).transpose(0, 2, 1, 3).reshape(NE, 128, 2048)
        m["w_all"] = wa.reshape(NE * 128, 6144)
    m["ws_gate"] = f(inp["ws_gate"][0])
    m["ws_up"] = f(inp["ws_up"][0])
    m["ws_down"] = f(inp["ws_down"][0])
    m["nfin_bc"] = f(np.tile(np.asarray(inp["norm_final"])[None, :], (128, 1)))
    return m


def _core_inputs(inp, b, shared):
    m = dict(shared)
    x = np.asarray(inp["x"][b], dtype=np.float32)
    m["xT"] = np.ascontiguousarray(x.T).reshape(8, 128, S)
    m["c_l"] = np.ascontiguousarray(np.asarray(inp["c"][b], dtype=np.float32).reshape(8, 128).T)
    return m


_NC_CACHE = {}


def kernel(**inputs):
    if "nc" not in _NC_CACHE:
        _NC_CACHE["nc"] = build_nc()
    nc = _NC_CACHE["nc"]
    shared = _shared_inputs(inputs)
    in_maps = [_core_inputs(inputs, b, shared) for b in range(8)]
    res = run_bass_kernel_spmd(nc, in_maps, core_ids=list(range(8)))
    return np.stack([np.asarray(r["out"], dtype=np.float32) for r in res.results], axis=0)


def phase_D(k):
    nc, sch = k.nc, k.sch
    with ExitStack() as st:
        sb, ps = k.sb, k.ps
        qf = sb(st, "qf", [128, 2, S], BF16)
        qb = sb(st, "qb", [128, 2, S], BF16)
        kf = sb(st, "kf", [128, 2, S], BF16)
        kb = sb(st, "kb", [128, 2, S], BF16)
        sg = sb(st, "sg", [128, 2, S], BF16)
        vv = sb(st, "vv", [128, NT, 256], BF16)
        sbs = sb(st, "sbs", [128, NT, 2, 256], BF16)
        oret = sb(st, "oret", [128, 2, S], BF16)
        Sm = sb(st, "Sm", [128, 2, 256], F32)
        Tm = sb(st, "Tm", [128, 2, 256], F32)
        sfb = [sb(st, f"sfb{i}", [128, 2, 256], BF16) for i in range(2)]
        kt = [sb(st, f"kt{i}", [128, 256], BF16) for i in range(2)]
        t1 = [sb(st, f"rt1{i}", [128, 128], BF16) for i in range(2)]
        t2 = [sb(st, f"rt2{i}", [128, 128], BF16) for i in range(2)]
        pt = [sb(st, f"rpt{i}", [128, 128], BF16) for i in range(2)]
        og = [sb(st, f"og{i}", [128, 2, 256], F32) for i in range(2)]
        osq = sb(st, "osq", [128, 2, 256], F32)
        mean = sb(st, "mean", [128, 256], F32)
        msq = sb(st, "msq", [128, 256], F32)
        rstd = sb(st, "rstd", [128, 256], F32)
        tn = [sb(st, f"tn{i}", [128, 256], F32) for i in range(2)]
        ptr = [ps(st, f"ptr{i}", [128, 256], BF16) for i in range(2)]
        pds = ps(st, "pds", [128, 2, 256])
        pS = [ps(st, f"pS{i}", [128, 256]) for i in range(2)]
        pO = [ps(st, f"pO{i}", [128, 2, 128]) for i in range(2)]
        pst = ps(st, "pst", [128, 2, 256])
        T = Tok
        t_qf, t_qb, t_kf, t_kb, t_sg, t_vv, t_oret, t_Sm, t_Tm = (T() for _ in range(9))
        t_sbs = [T() for _ in range(NT)]
        t_sfb, t_kt, t_t1, t_t2, t_pt, t_og = ([T(), T()] for _ in range(6))
        t_osq, t_mean, t_msq, t_rstd = T(), T(), T(), T()
        t_tn = [T(), T()]
        t_ptr, t_pS, t_pO = [T(), T()], [T(), T()], [T(), T()]
        t_pds, t_pst = T(), T()
        RVv = k.RV.rearrange("(n p) c -> p n c", p=128)
        cnt = {"kt": 0}

        def ktrans(src, t_src, n):
            i = cnt["kt"] % 2
            cnt["kt"] += 1
            for dc in range(2):
                sch.op("pe", lambda i=i, dc=dc, n=n: nc.tensor.transpose(
                    out=ptr[i][:, dc * 128:(dc + 1) * 128], in_=src[:, dc, n * 128:(n + 1) * 128], identity=k.ident_b),
                    reads=[t_src, k.t_cb], writes=[t_ptr[i]])
            sch.op("act", lambda i=i: nc.scalar.copy(out=kt[i][:, :], in_=ptr[i][:, :]),
                   reads=[t_ptr[i]], writes=[t_kt[i]])
            return i

        def dstate(i, n):
            for dc in range(2):
                sch.op("pe", lambda i=i, dc=dc, n=n: nc.tensor.matmul(
                    pds[:, dc, :], lhsT=kt[i][:, dc * 128:(dc + 1) * 128], rhs=vv[:, n, :], start=True, stop=True),
                    reads=[t_kt[i], t_vv], writes=[t_pds])

        def supdate(first, cd, bf_out, t_bf, cast_eng="act"):
            if first:
                sch.op("dve", lambda: nc.vector.tensor_scalar(
                    out=Sm[:, :, :], in0=pds[:, :, :], scalar1=cd, scalar2=None, op0=ALU.mult),
                    reads=[t_pds, k.t_dec], writes=[t_Sm])
            else:
                sch.op("dve", lambda: nc.vector.scalar_tensor_tensor(
                    out=Sm[:, :, :], in0=pds[:, :, :], scalar=cd, in1=Tm[:, :, :], op0=ALU.mult, op1=ALU.add),
                    reads=[t_pds, t_Tm, k.t_dec], writes=[t_Sm])
            sch.op("act", lambda: nc.scalar.activation(out=Tm[:, :, :], in_=Sm[:, :, :], func=AF.Copy, scale=cd),
                   reads=[t_Sm, k.t_dec], writes=[t_Tm])
            if cast_eng == "act":
                sch.op("act", lambda: nc.scalar.copy(out=bf_out, in_=Sm[:, :, :]), reads=[t_Sm], writes=[t_bf])
            else:
                sch.op("pool", lambda: nc.gpsimd.tensor_copy(out=bf_out, in_=Sm[:, :, :]), reads=[t_Sm], writes=[t_bf])

        for h in range(4):
            sch.op("sp", lambda h=h: nc.sync.dma_start(
                out=kb[:, :, :], in_=k.KB[2 * h:2 * h + 2, :, :].rearrange("c p t -> p c t")), writes=[t_kb], dma=True)
            sch.op("sp", lambda h=h: nc.sync.dma_start(out=vv[:, :, :], in_=RVv[:, :, 256 * h:256 * h + 256]),
                   writes=[t_vv], dma=True)
            for (buf, tk, src) in ((kf, t_kf, k.KF), (qf, t_qf, k.QF), (qb, t_qb, k.QB), (sg, t_sg, k.SG)):
                sch.op("sp", lambda buf=buf, src=src, h=h: nc.sync.dma_start(
                    out=buf[:, :, :], in_=src[2 * h:2 * h + 2, :, :].rearrange("c p t -> p c t")),
                    writes=[tk], dma=True)
            cdF = k.dec[:, 8 + h:9 + h]
            cdB = k.dec[:, 12 + h:13 + h]
            inext = ktrans(kb, t_kb, NT - 1)
            for n in range(NT - 1, 0, -1):
                i = inext
                if n - 1 >= 1:
                    inext = ktrans(kb, t_kb, n - 1)
                dstate(i, n)
                supdate(n == NT - 1, cdB, sbs[:, n - 1, :, :], t_sbs[n - 1], cast_eng="pool")

            def s_part(n):
                p2 = n % 2
                c0, c1 = n * 128, (n + 1) * 128
                for (col, ksrc, tks, qsrc, tqs) in ((0, kf, t_kf, qf, t_qf), (1, kb, t_kb, qb, t_qb)):
                    for dc in range(2):
                        sch.op("pe", lambda p2=p2, col=col, ksrc=ksrc, qsrc=qsrc, dc=dc, c0=c0, c1=c1: nc.tensor.matmul(
                            pS[p2][:, col * 128:(col + 1) * 128], lhsT=ksrc[:, dc, c0:c1], rhs=qsrc[:, dc, c0:c1],
                            start=(dc == 0), stop=(dc == 1)),
                            reads=[tks, tqs], writes=[t_pS[p2]])
                sch.op("dve", lambda p2=p2: nc.vector.tensor_tensor(
                    out=t1[p2][:, :], in0=pS[p2][:, 0:128], in1=k.maskF, op=ALU.mult),
                    reads=[t_pS[p2], k.t_cb], writes=[t_t1[p2]])
                sch.op("dve", lambda p2=p2: nc.vector.tensor_tensor(
                    out=t2[p2][:, :], in0=pS[p2][:, 128:256], in1=k.maskB, op=ALU.mult),
                    reads=[t_pS[p2], k.t_cb], writes=[t_t2[p2]])
                sch.op("dve", lambda p2=p2: nc.vector.tensor_tensor(
                    out=pt[p2][:, :], in0=t1[p2][:, :], in1=t2[p2][:, :], op=ALU.add),
                    reads=[t_t1[p2], t_t2[p2]], writes=[t_pt[p2]])

            s_part(0)
            ki = ktrans(kf, t_kf, 0)
            for n in range(NT):
                p2 = n % 2
                c0, c1 = n * 128, (n + 1) * 128
                ki_cur = ki
                if n + 1 < NT:
                    s_part(n + 1)
                    if n + 1 < NT - 1:
                        ki = ktrans(kf, t_kf, n + 1)
                if n < NT - 1:
                    dstate(ki_cur, n)
                sfi = n % 2
                for ec in range(2):
                    e0, e1 = ec * 128, (ec + 1) * 128
                    mms = [(vv[:, n, e0:e1], pt[p2][:, :], [t_vv, t_pt[p2]])]
                    if n > 0:
                        for dc in range(2):
                            mms.append((sfb[sfi][:, dc, e0:e1], qf[:, dc, c0:c1], [t_sfb[sfi], t_qf]))
                    if n < NT - 1:
                        for dc in range(2):
                            mms.append((sbs[:, n, dc, e0:e1], qb[:, dc, c0:c1], [t_sbs[n], t_qb]))
                    for mi, (lt, rh, rd) in enumerate(mms):
                        sch.op("pe", lambda p2=p2, ec=ec, lt=lt, rh=rh, mi=mi, last=(mi == len(mms) - 1): nc.tensor.matmul(
                            pO[p2][:, ec, :], lhsT=lt, rhs=rh, start=(mi == 0), stop=last),
                            reads=rd, writes=[t_pO[p2]])
                if n < NT - 1:
                    supdate(n == 0, cdF, sfb[(n + 1) % 2][:, :, :], t_sfb[(n + 1) % 2])
                gi = (n // 2) % 2
                gp = n % 2
                sch.op("act", lambda p2=p2, gi=gi, gp=gp: nc.scalar.copy(
                    out=og[gi][:, :, gp * 128:(gp + 1) * 128], in_=pO[p2][:, :, :]),
                    reads=[t_pO[p2]], writes=[t_og[gi]])
                if gp == 1:
                    g0 = (n - 1) * 128
                    sch.op("act", lambda gi=gi: nc.scalar.activation(out=osq[:, :, :], in_=og[gi][:, :, :], func=AF.Square),
                           reads=[t_og[gi]], writes=[t_osq])
                    for (sidx, src, tsrc) in ((0, og[gi], t_og[gi]), (1, osq, t_osq)):
                        for ec in range(2):
                            sch.op("pe", lambda sidx=sidx, src=src, ec=ec: nc.tensor.matmul(
                                pst[:, sidx, :], lhsT=k.ones_f, rhs=src[:, ec, :], start=(ec == 0), stop=(ec == 1)),
                                reads=[tsrc, k.t_cf], writes=[t_pst])
                    sch.op("act", lambda: nc.scalar.activation(out=mean[:, :], in_=pst[:, 0, :], func=AF.Copy, scale=1.0 / 256),
                           reads=[t_pst], writes=[t_mean])
                    sch.op("dve", lambda: nc.vector.tensor_tensor(out=msq[:, :], in0=mean[:, :], in1=mean[:, :], op=ALU.mult),
                           reads=[t_mean], writes=[t_msq])
                    sch.op("dve", lambda: nc.vector.scalar_tensor_tensor(
                        out=rstd[:, :], in0=pst[:, 1, :], scalar=1.0 / 256, in1=msq[:, :], op0=ALU.mult, op1=ALU.subtract),
                        reads=[t_pst, t_msq], writes=[t_rstd])
                    sch.op("act", lambda: nc.scalar.activation(out=rstd[:, :], in_=rstd[:, :], func=AF.Sqrt, bias=EPS, scale=1.0),
                           reads=[t_rstd], writes=[t_rstd])
                    sch.op("dve", lambda: nc.vector.reciprocal(out=rstd[:, :], in_=rstd[:, :]),
                           reads=[t_rstd], writes=[t_rstd])
                    for ec in range(2):
                        sch.op("pool", lambda gi=gi, ec=ec: nc.gpsimd.tensor_tensor(
                            out=tn[ec][:, :], in0=og[gi][:, ec, :], in1=mean[:, :], op=ALU.subtract),
                            reads=[t_og[gi], t_mean], writes=[t_tn[ec]])
                        sch.op("pool", lambda ec=ec: nc.gpsimd.tensor_tensor(
                            out=tn[ec][:, :], in0=tn[ec][:, :], in1=rstd[:, :], op=ALU.mult),
                            reads=[t_tn[ec], t_rstd], writes=[t_tn[ec]])
                        sch.op("dve", lambda ec=ec, g0=g0: nc.vector.tensor_tensor(
                            out=oret[:, ec, g0:g0 + 256], in0=tn[ec][:, :], in1=sg[:, ec, g0:g0 + 256], op=ALU.mult),
                            reads=[t_tn[ec], t_sg], writes=[t_oret])
            for ec in range(2):
                sch.op("sp", lambda h=h, ec=ec: nc.sync.dma_start(out=k.ORET[2 * h + ec, :, :], in_=oret[:, ec, :]),
                       reads=[t_oret], dma=True)
        sch.flush("D")


def phase_E(k):
    nc, sch = k.nc, k.sch
    with ExitStack() as st:
        sb, ps = k.sb, k.ps
        aq = [sb(st, f"aq{i}", [128, S], BF16) for i in range(2)]
        ak = [sb(st, f"ak{i}", [128, S], BF16) for i in range(2)]
        bst = sb(st, "bst", [128, 6, 384], F32)
        eb = [sb(st, f"eb{i}", [128, 6, 384], BF16) for i in range(2)]
        vg = [sb(st, f"vg{i}", [128, NT, 128], BF16) for i in range(2)]
        accn = sb(st, "accn", [128, S], F32)
        accz = sb(st, "accz", [128, S], F32)
        ost = sb(st, "ost", [128, S], BF16)
        esb = [sb(st, f"esb{i}", [128, 384], BF16) for i in range(4)]
        ptb = [sb(st, f"ptb{i}", [128, 384], BF16) for i in range(4)]
        pS = [ps(st, f"apS{i}", [128, 512]) for i in range(4)]
        pO = [ps(st, f"apO{i}", [128, 128]) for i in range(2)]
        pZ = [ps(st, f"apZ{i}", [128, 128]) for i in range(2)]
        T = Tok
        t_bst, t_accn, t_accz, t_ost = (T() for _ in range(4))
        t_aq, t_ak, t_eb, t_vg = ([T(), T()] for _ in range(4))
        t_esb = [T() for _ in range(4)]
        t_ptb = [T() for _ in range(4)]
        t_pS = [T() for _ in range(4)]
        t_pO, t_pZ = [T(), T()], [T(), T()]
        ones_b = k.cb[:, 896:960]
        RS = (1, 4, 16)
        groups = [(hp, g) for hp in range(8) for g in range(3)]

        def load_group(gi):
            hp, g = groups[gi]
            hb = hp % 2
            if g == 0:
                sch.op("sp", lambda: nc.sync.dma_start(out=aq[hb][:, :], in_=k.AQ[hp, :, :]), writes=[t_aq[hb]], dma=True)
                sch.op("sp", lambda: nc.sync.dma_start(out=ak[hb][:, :], in_=k.AK[hp, :, :]), writes=[t_ak[hb]], dma=True)
                sch.op("sp", lambda: nc.sync.dma_start(
                    out=bst[:, :, :], in_=k.t5b[hp, :, :].rearrange("p (a n) -> p a n", a=6)), writes=[t_bst], dma=True)
                sch.op("act", lambda: nc.scalar.activation(out=bst[:, :, :], in_=bst[:, :, :], func=AF.Exp),
                       reads=[t_bst], writes=[t_bst])
                for a in range(6):
                    sch.op("dve", lambda a=a: nc.vector.tensor_tensor(
                        out=eb[hb][:, a, :], in0=bst[:, a, :], in1=k.attmask, op=ALU.mult),
                        reads=[t_bst, k.t_cb], writes=[t_eb[hb]])
            r = RS[g]
            nb = NT // r
            vi = gi % 2
            src = k.AV[:, 128 * hp:128 * hp + 128].rearrange("(b kk c) f -> kk c b f", kk=128, c=r)
            for c in range(r):
                sch.op("sp", lambda c=c: nc.sync.dma_start(
                    out=vg[vi][:, c * nb:(c + 1) * nb, :], in_=src[:, c, :, :]), writes=[t_vg[vi]], dma=True)

        blocks = []
        for gi, (hp, g) in enumerate(groups):
            r = RS[g]
            nb = NT // r
            for c in range(r):
                for b in range(nb):
                    blocks.append(dict(gi=gi, hp=hp, g=g, r=r, nb=nb, c=c, b=b, first=(c == 0 and b == 0),
                                       last=(g == 2 and c == r - 1 and b == nb - 1)))

        def s_part(i):
            bl = blocks[i]
            hp, g, r, nb, c, b = bl["hp"], bl["g"], bl["r"], bl["nb"], bl["c"], bl["b"]
            hb = hp % 2
            aqv = aq[hb][:, :].rearrange("p (m r) -> p r m", r=r)
            akv = ak[hb][:, :].rearrange("p (m r) -> p r m", r=r)
            tl = [t for t in (-1, 0, 1) if 0 <= b + t < nb]
            cmin, cmax = (tl[0] + 1) * 128, (tl[-1] + 2) * 128
            for hh in range(2):
                si = (i % 2) * 2 + hh
                r0, r1 = 64 * hh, 64 * hh + 64
                for t in tl:
                    sch.op("pe", lambda si=si, t=t, r0=r0, r1=r1: nc.tensor.matmul(
                        pS[si][:, (t + 1) * 128:(t + 2) * 128],
                        lhsT=akv[r0:r1, c, 128 * (b + t):128 * (b + t + 1)],
                        rhs=aqv[r0:r1, c, 128 * b:128 * (b + 1)], start=True, stop=True),
                        reads=[t_ak[hb], t_aq[hb]], writes=[t_pS[si]])
                sch.op("act", lambda si=si: nc.scalar.activation(
                    out=esb[si][:, cmin:cmax], in_=pS[si][:, cmin:cmax], func=AF.Exp, scale=0.125),
                    reads=[t_pS[si]], writes=[t_esb[si]])
                sch.op("dve", lambda si=si, a=g * 2 + hh: nc.vector.tensor_tensor(
                    out=ptb[si][:, cmin:cmax], in0=esb[si][:, cmin:cmax], in1=eb[hb][:, a, cmin:cmax], op=ALU.mult),
                    reads=[t_esb[si], t_eb[hb]], writes=[t_ptb[si]])

        def pv_part(i):
            bl = blocks[i]
            hp, g, r, nb, c, b, gi = bl["hp"], bl["g"], bl["r"], bl["nb"], bl["c"], bl["b"], bl["gi"]
            vi = gi % 2
            tl = [t for t in (-1, 0, 1) if 0 <= b + t < nb]
            oi = i % 2
            for hh in range(2):
                si = (i % 2) * 2 + hh
                r0, r1 = 64 * hh, 64 * hh + 64
                for ti, t in enumerate(tl):
                    sch.op("pe", lambda si=si, t=t, r0=r0, r1=r1, blk=c * nb + b + t, ti=ti, nt=len(tl): nc.tensor.matmul(
                        pO[oi][r0:r1, :], lhsT=vg[vi][:, blk, r0:r1], rhs=ptb[si][:, (t + 1) * 128:(t + 2) * 128],
                        start=(ti == 0), stop=(ti == nt - 1)),
                        reads=[t_vg[vi], t_ptb[si]], writes=[t_pO[oi]])
                for ti, t in enumerate(tl):
                    sch.op("pe", lambda si=si, t=t, r0=r0, r1=r1, ti=ti, nt=len(tl): nc.tensor.matmul(
                        pZ[oi][r0:r1, :], lhsT=ones_b, rhs=ptb[si][:, (t + 1) * 128:(t + 2) * 128],
                        start=(ti == 0), stop=(ti == nt - 1)),
                        reads=[k.t_cb, t_ptb[si]], writes=[t_pZ[oi]])
            dn = accn[:, :].rearrange("p (m r) -> p r m", r=r)[:, c, 128 * b:128 * (b + 1)]
            dz = accz[:, :].rearrange("p (m r) -> p r m", r=r)[:, c, 128 * b:128 * (b + 1)]
            if g == 0:
                sch.op("act", lambda: nc.scalar.copy(out=dn, in_=pO[oi][:, :]), reads=[t_pO[oi]], writes=[t_accn])
                sch.op("act", lambda: nc.scalar.copy(out=dz, in_=pZ[oi][:, :]), reads=[t_pZ[oi]], writes=[t_accz])
            else:
                sch.op("dve", lambda: nc.vector.tensor_tensor(out=dn, in0=pO[oi][:, :], in1=dn, op=ALU.add),
                       reads=[t_pO[oi], t_accn], writes=[t_accn])
                sch.op("dve", lambda: nc.vector.tensor_tensor(out=dz, in0=pZ[oi][:, :], in1=dz, op=ALU.add),
                       reads=[t_pZ[oi], t_accz], writes=[t_accz])
            if bl["last"]:
                for q4 in range(4):
                    sl = slice(q4 * 1024, (q4 + 1) * 1024)
                    sch.op("dve", lambda sl=sl: nc.vector.reciprocal(out=accz[:, sl], in_=accz[:, sl]),
                           reads=[t_accz], writes=[t_accz])
                    sch.op("pool", lambda sl=sl: nc.gpsimd.tensor_tensor(out=ost[:, sl], in0=accn[:, sl], in1=accz[:, sl], op=ALU.mult),
                           reads=[t_accn, t_accz], writes=[t_ost])
                sch.op("sp", lambda: nc.sync.dma_start(out=k.OATT[hp, :, :], in_=ost[:, :]), reads=[t_ost], dma=True)

        load_group(0)
        load_group(1)
        s_part(0)
        for i in range(len(blocks)):
            if i + 1 < len(blocks):
                s_part(i + 1)
            pv_part(i)
            if i + 1 < len(blocks):
                nb_ = blocks[i + 1]
                if nb_["first"] and nb_["gi"] + 1 < len(groups):
                    load_group(nb_["gi"] + 1)
        sch.flush("E")


def phase_F0(k, st):
    nc, sch = k.nc, k.sch
    sb = k.sb
    k.wru = sb(st, "wru", [128, 8, D], BF16)
    k.wau = sb(st, "wau", [128, 8, D], BF16)
    k.wo = sb(st, "wo", [128, 8, D], BF16)
    k.wsg = sb(st, "wsg", [128, 8, 256], BF16)
    k.wsu = sb(st, "wsu", [128, 8, 256], BF16)
    k.wsd = sb(st, "wsd", [128, 2, D], BF16)
    k.wr = sb(st, "wr", [128, 8, NE], F32)
    k.rbias = sb(st, "rbias", [128, NE], F32)
    k.base = sb(st, "base", [128, NE], F32)
    k.cg = sb(st, "cg", [128, 1024], F32)
    k.t_cg = Tok()
    k.jrow = k.cg[:, 0:512]
    k.eplus1 = k.cg[:, 512:768]
    k.pcol = k.cg[:, 768:769]
    k.t_wF = Tok()
    k.t_base = Tok()
    with ExitStack() as s2:
        stg = [sb(s2, f"wstg{i}", [128, 4, D], F32) for i in range(2)]
        t_stg = [Tok(), Tok()]
        jobs = []
        for (dst, src) in ((k.wru, k.w_ret_up), (k.wau, k.w_att_up), (k.wo, k.w_o)):
            sv = src.rearrange("(kk p) n -> p kk n", p=128)
            for hlf in range(2):
                jobs.append((dst[:, hlf * 4:(hlf + 1) * 4, :], sv[:, hlf * 4:(hlf + 1) * 4, :], None))
        for (dst, src) in ((k.wsg, k.ws_gate), (k.wsu, k.ws_up)):
            sv = src.rearrange("(kk p) n -> p kk n", p=128)
            jobs.append((dst[:, :, :], sv[:, :, :], (8, 256)))
        jobs.append((k.wsd[:, :, :], k.ws_down.rearrange("(kk p) n -> p kk n", p=128), (2, D)))
        for ji, (dst, src, shp) in enumerate(jobs):
            b = ji % 2
            if shp is None:
                sview = stg[b][:, :, :]
            elif shp == (8, 256):
                sview = stg[b][:, :, :].rearrange("p a (b c) -> p (a b) c", c=256)[:, 0:8, :]
            else:
                sview = stg[b][:, 0:2, :]
            sch.op("sp", lambda sview=sview, src=src: nc.sync.dma_start(out=sview, in_=src), writes=[t_stg[b]], dma=True)
            eng = ("pool", "act", "dve")[ji % 3]
            if eng == "pool":
                sch.op("pool", lambda dst=dst, sview=sview: nc.gpsimd.tensor_copy(out=dst, in_=sview), reads=[t_stg[b]], writes=[k.t_wF])
            elif eng == "act":
                sch.op("act", lambda dst=dst, sview=sview: nc.scalar.copy(out=dst, in_=sview), reads=[t_stg[b]], writes=[k.t_wF])
            else:
                sch.op("dve", lambda dst=dst, sview=sview: nc.vector.tensor_copy(out=dst, in_=sview), reads=[t_stg[b]], writes=[k.t_wF])
        sch.op("sp", lambda: nc.sync.dma_start(out=k.wr[:, :, :], in_=k.w_router.rearrange("(kk p) n -> p kk n", p=128)),
               writes=[k.t_wF], dma=True)
        sch.op("sp", lambda: nc.sync.dma_start(out=k.rbias[:, :], in_=k.rbias_bc[:, :]), writes=[k.t_wF], dma=True)
        sch.op("pool", lambda: nc.gpsimd.memset(k.base[:, :], 0.0), writes=[k.t_base])
        sch.op("sp", lambda: nc.sync.dma_start(out=k.cg[:, :], in_=k.cst_g[:, :]), writes=[k.t_cg], dma=True)
        sch.flush("F0")


def phase_F1(k):
    nc, sch = k.nc, k.sch
    with ExitStack() as st:
        sb, ps = k.sb, k.ps
        oret_t = sb(st, "oret_t", [128, 8, 512], BF16)
        oatt_t = sb(st, "oatt_t", [128, 8, 512], BF16)
        gr_t = sb(st, "gr_t", [128, 8, 512], BF16)
        ga_t = sb(st, "ga_t", [128, 8, 512], BF16)
        x_t = sb(st, "x_t", [128, 8, 512], F32)
        merged = sb(st, "merged", [128, 8, 512], BF16)
        x1T = x_t
        h2T = sb(st, "h2T", [128, 8, 512], F32)
        h2Tb = sb(st, "h2Tb", [128, 8, 512], BF16)
        rs = sb(st, "rsF", [128, 512], F32)
        tA = [sb(st, f"tA{i}", [128, 512], F32) for i in range(2)]
        tB = [sb(st, f"tB{i}", [128, 512], F32) for i in range(2)]
        hid = sb(st, "hid", [128, 2, 512], BF16)
        x1tok = [sb(st, f"x1tok{i}", [128, D], F32) for i in range(1)] * 2
        h2tok = [sb(st, f"h2tok{i}", [128, D], BF16) for i in range(2)]
        xs = [sb(st, f"xs{i}", [128, D], F32) for i in range(1)] * 2
        sc = sb(st, "sc", [128, NE], F32)
        ch = sb(st, "ch", [128, NE], F32)
        chm = sb(st, "chm", [128, NE], F32)
        sel = sb(st, "sel", [128, NE], F32)
        selb = sb(st, "selb", [128, NE], BF16)
        gsel = sb(st, "gsel", [128, NE], F32)
        sval = sb(st, "sval", [128, NE], F32)
        junk = sb(st, "junk", [128, NE], F32)
        m8 = sb(st, "m8", [128, 8, 8], F32)
        sm = sb(st, "sm", [128, 64], F32)
        banks = [ps(st, f"fb{i}", [128, 512]) for i in range(7)]
        pH = ps(st, "fpH", [128, D], BF16)
        T = Tok
        t_oret, t_oatt, t_gr, t_ga, t_x, t_merged, t_x1T, t_h2T, t_h2Tb, t_rs, t_hid = (T() for _ in range(11))
        t_x1T = t_x
        t_tA, t_tB = [T(), T()], [T(), T()]
        t_x1tok, t_h2tok, t_xs = [T()] * 2, [T(), T()], [T()] * 2
        t_sc, t_ch, t_chm, t_sel, t_selb, t_gsel, t_sval, t_junk, t_m8, t_sm = (T() for _ in range(10))
        t_banks = [T() for _ in range(7)]
        t_pH = T()
        cnt = {"b": 0, "tA": 0, "tB": 0, "s": 0}

        def nbank():
            i = cnt["b"] % 7
            cnt["b"] += 1
            return i

        def fm(n_, c0):
            return n_[:, :, c0:c0 + 512].rearrange("c p t -> p c t")

        pending = []
        for Tt in range(8):
            c0 = Tt * 512
            for (buf, tk, src) in ((oret_t, t_oret, k.ORET), (oatt_t, t_oatt, k.OATT), (gr_t, t_gr, k.GR),
                                   (ga_t, t_ga, k.GA), (x_t, t_x, k.xT)):
                sch.op("sp", lambda buf=buf, src=src, c0=c0: nc.sync.dma_start(out=buf[:, :, :], in_=fm(src, c0)),
                       writes=[tk], dma=True)
            for n_ in range(8):
                n0, n1 = n_ * 128, (n_ + 1) * 128
                b1, b2 = nbank(), nbank()
                for (bi, w, act, tact) in ((b1, k.wru, oret_t, t_oret), (b2, k.wau, oatt_t, t_oatt)):
                    for kk in range(8):
                        sch.op("pe", lambda bi=bi, w=w, act=act, kk=kk, n0=n0, n1=n1: nc.tensor.matmul(
                            banks[bi][:, :], lhsT=w[:, kk, n0:n1], rhs=act[:, kk, :], start=(kk == 0), stop=(kk == 7)),
                            reads=[k.t_wF, tact], writes=[t_banks[bi]])
                ia = cnt["tA"] % 2
                cnt["tA"] += 1
                sch.op("dve", lambda b1=b1, ia=ia, n_=n_: nc.vector.tensor_tensor(
                    out=tA[ia][:, :], in0=banks[b1][:, :], in1=gr_t[:, n_, :], op=ALU.mult),
                    reads=[t_banks[b1], t_gr], writes=[t_tA[ia]])
                sch.op("dve", lambda b2=b2, ia=ia, n_=n_: nc.vector.tensor_tensor(
                    out=tB[ia][:, :], in0=banks[b2][:, :], in1=ga_t[:, n_, :], op=ALU.mult),
                    reads=[t_banks[b2], t_ga], writes=[t_tB[ia]])
                sch.op("dve", lambda ia=ia, n_=n_: nc.vector.tensor_tensor(
                    out=merged[:, n_, :], in0=tA[ia][:, :], in1=tB[ia][:, :], op=ALU.add),
                    reads=[t_tA[ia], t_tB[ia]], writes=[t_merged])
            for n_ in range(8):
                n0, n1 = n_ * 128, (n_ + 1) * 128
                bi = nbank()
                for kk in range(8):
                    sch.op("pe", lambda bi=bi, kk=kk, n0=n0, n1=n1: nc.tensor.matmul(
                        banks[bi][:, :], lhsT=k.wo[:, kk, n0:n1], rhs=merged[:, kk, :], start=(kk == 0), stop=(kk == 7)),
                        reads=[k.t_wF, t_merged], writes=[t_banks[bi]])
                sch.op("dve", lambda bi=bi, n_=n_: nc.vector.scalar_tensor_tensor(
                    out=x1T[:, n_, :], in0=banks[bi][:, :], scalar=k.modv[:, 16 + n_:17 + n_], in1=x_t[:, n_, :],
                    op0=ALU.mult, op1=ALU.add),
                    reads=[t_banks[bi], t_x, k.t_modv], writes=[t_x1T])
            sch.op("act", lambda: nc.scalar.activation(out=h2T[:, :, :], in_=x1T[:, :, :], func=AF.Square),
                   reads=[t_x1T], writes=[t_h2T])
            bi = nbank()
            for kk in range(8):
                sch.op("pe", lambda bi=bi, kk=kk: nc.tensor.matmul(
                    banks[bi][:, :], lhsT=k.ones_f, rhs=h2T[:, kk, :], start=(kk == 0), stop=(kk == 7)),
                    reads=[t_h2T, k.t_cf], writes=[t_banks[bi]])
            sch.op("act", lambda bi=bi: nc.scalar.activation(out=rs[:, :], in_=banks[bi][:, :], func=AF.Sqrt,
                                                              scale=1.0 / D, bias=EPS),
                   reads=[t_banks[bi]], writes=[t_rs])
            sch.op("dve", lambda: nc.vector.reciprocal(out=rs[:, :], in_=rs[:, :]), reads=[t_rs], writes=[t_rs])
            for kk in range(8):
                ia = cnt["tA"] % 2
                cnt["tA"] += 1
                sch.op("dve", lambda kk=kk, ia=ia: nc.vector.scalar_tensor_tensor(
                    out=tA[ia][:, :], in0=x1T[:, kk, :], scalar=k.modv[:, 56 + kk:57 + kk], in1=rs[:, :],
                    op0=ALU.mult, op1=ALU.mult),
                    reads=[t_x1T, t_rs, k.t_modv], writes=[t_tA[ia]])
                sch.op("act", lambda kk=kk, ia=ia: nc.scalar.activation(
                    out=h2T[:, kk, :], in_=tA[ia][:, :], func=AF.Identity, bias=k.modv[:, 24 + kk:25 + kk], scale=1.0),
                    reads=[t_tA[ia], k.t_modv], writes=[t_h2T])
                sch.op("act", lambda kk=kk: nc.scalar.copy(out=h2Tb[:, kk, :], in_=h2T[:, kk, :]),
                       reads=[t_h2T], writes=[t_h2Tb])
            for jc in range(2):
                j0, j1 = jc * 128, (jc + 1) * 128
                bg, bu = nbank(), nbank()
                for (bi, w) in ((bg, k.wsg), (bu, k.wsu)):
                    for kk in range(8):
                        sch.op("pe", lambda bi=bi, w=w, kk=kk, j0=j0, j1=j1: nc.tensor.matmul(
                            banks[bi][:, :], lhsT=w[:, kk, j0:j1], rhs=h2Tb[:, kk, :], start=(kk == 0), stop=(kk == 7)),
                            reads=[k.t_wF, t_h2Tb], writes=[t_banks[bi]])
                ia = cnt["tA"] % 2
                cnt["tA"] += 1
                sch.op("act", lambda bg=bg, ia=ia: nc.scalar.activation(out=tA[ia][:, :], in_=banks[bg][:, :], func=AF.Silu),
                       reads=[t_banks[bg]], writes=[t_tA[ia]])
                sch.op("dve", lambda bu=bu, ia=ia, jc=jc: nc.vector.tensor_tensor(
                    out=hid[:, jc, :], in0=banks[bu][:, :], in1=tA[ia][:, :], op=ALU.mult),
                    reads=[t_banks[bu], t_tA[ia]], writes=[t_hid])
            for s in range(4):
                gt = Tt * 4 + s
                s0, s1 = s * 128, (s + 1) * 128
                si = cnt["s"] % 2
                cnt["s"] += 1
                for hf in range(2):
                    bi = nbank()
                    for q in range(4):
                        kk = hf * 4 + q
                        sch.op("pe", lambda bi=bi, q=q, kk=kk, s0=s0, s1=s1: nc.tensor.transpose(
                            out=banks[bi][:, q * 128:(q + 1) * 128], in_=x1T[:, kk, s0:s1], identity=k.ident_f),
                            reads=[t_x1T, k.t_cf], writes=[t_banks[bi]])
                    sch.op("act", lambda bi=bi, si=si, hf=hf: nc.scalar.copy(
                        out=x1tok[si][:, hf * 512:(hf + 1) * 512], in_=banks[bi][:, :]),
                        reads=[t_banks[bi]], writes=[t_x1tok[si]])
                for kk in range(8):
                    sch.op("pe", lambda kk=kk, s0=s0, s1=s1: nc.tensor.transpose(
                        out=pH[:, kk * 128:(kk + 1) * 128], in_=h2Tb[:, kk, s0:s1], identity=k.ident_b),
                        reads=[t_h2Tb, k.t_cb], writes=[t_pH])
                sch.op("dve", lambda si=si: nc.vector.tensor_copy(out=h2tok[si][:, :], in_=pH[:, :]),
                       reads=[t_pH], writes=[t_h2tok[si]])
                bl = nbank()
                for kk in range(8):
                    sch.op("pe", lambda bl=bl, kk=kk, s0=s0, s1=s1: nc.tensor.matmul(
                        banks[bl][:, 0:NE], lhsT=h2T[:, kk, s0:s1], rhs=k.wr[:, kk, :], start=(kk == 0), stop=(kk == 7)),
                        reads=[t_h2T, k.t_wF], writes=[t_banks[bl]])
                V = nc.vector
                while pending:
                    pending.pop(0)()
                for hf in range(2):
                    bi = nbank()
                    for jc in range(2):
                        sch.op("pe", lambda bi=bi, jc=jc, hf=hf, s0=s0, s1=s1: nc.tensor.matmul(
                            banks[bi][:, :], lhsT=hid[:, jc, s0:s1], rhs=k.wsd[:, jc, hf * 512:(hf + 1) * 512],
                            start=(jc == 0), stop=(jc == 1)),
                            reads=[t_hid, k.t_wF], writes=[t_banks[bi]])
                    ib = cnt["tB"] % 2
                    cnt["tB"] += 1
                    sch.op("dve", lambda bi=bi, ib=ib, hf=hf: V.tensor_tensor(
                        out=tB[ib][:, :], in0=banks[bi][:, :], in1=k.gfbc[:, hf * 512:(hf + 1) * 512], op=ALU.mult),
                        reads=[t_banks[bi], k.t_gfbc], writes=[t_tB[ib]])
                    sch.op("pool", lambda ib=ib, si=si, hf=hf: nc.gpsimd.tensor_tensor(
                        out=xs[si][:, hf * 512:(hf + 1) * 512], in0=tB[ib][:, :], in1=x1tok[si][:, hf * 512:(hf + 1) * 512],
                        op=ALU.add),
                        reads=[t_tB[ib], t_x1tok[si]], writes=[t_xs[si]])
                sch.op("sp", lambda gt=gt, si=si: nc.sync.dma_start(out=k.XS[gt * 128:(gt + 1) * 128, :], in_=xs[si][:, :]),
                       reads=[t_xs[si]], dma=True)
                sch.op("act", lambda bl=bl: nc.scalar.activation(out=sc[:, :], in_=banks[bl][:, 0:NE], func=AF.Sigmoid),
                       reads=[t_banks[bl]], writes=[t_sc])
                sch.op("dve", lambda: V.tensor_tensor(out=ch[:, :], in0=sc[:, :], in1=k.rbias[:, :], op=ALU.add),
                       reads=[t_sc, k.t_wF], writes=[t_ch])
                for g8 in range(8):
                    sch.op("dve", lambda g8=g8: V.max(out=m8[:, g8, :], in_=ch[:, g8 * 32:(g8 + 1) * 32]),
                           reads=[t_ch], writes=[t_m8])
                sch.op("dve", lambda: V.tensor_tensor(out=sm[:, 0:8], in0=m8[:, :, 0], in1=m8[:, :, 1], op=ALU.add),
                       reads=[t_m8], writes=[t_sm])
                sch.op("dve", lambda: V.max(out=sm[:, 8:16], in_=sm[:, 0:8]), reads=[t_sm], writes=[t_sm])
                sch.op("dve", lambda: V.tensor_scalar(out=sm[:, 16:24], in0=sm[:, 0:8], scalar1=sm[:, 11:12], scalar2=None,
                                                      op0=ALU.is_ge), reads=[t_sm], writes=[t_sm])
                sch.op("dve", lambda: V.tensor_scalar(out=sm[:, 24:32], in0=sm[:, 16:24], scalar1=1e9, scalar2=-1e9,
                                                      op0=ALU.mult, op1=ALU.add), reads=[t_sm], writes=[t_sm])
                for g8 in range(8):
                    sch.op("dve", lambda g8=g8: V.tensor_scalar(
                        out=chm[:, g8 * 32:(g8 + 1) * 32], in0=ch[:, g8 * 32:(g8 + 1) * 32],
                        scalar1=sm[:, 16 + g8:17 + g8], scalar2=sm[:, 24 + g8:25 + g8], op0=ALU.mult, op1=ALU.add),
                        reads=[t_ch, t_sm], writes=[t_chm])
                sch.op("dve", lambda: V.max(out=sm[:, 32:40], in_=chm[:, :]), reads=[t_chm], writes=[t_sm])
                sch.op("dve", lambda: V.tensor_scalar(out=sel[:, :], in0=chm[:, :], scalar1=sm[:, 39:40], scalar2=None,
                                                      op0=ALU.is_ge), reads=[t_chm, t_sm], writes=[t_sel])
                sch.op("act", lambda: nc.scalar.copy(out=selb[:, :], in_=sel[:, :]), reads=[t_sel], writes=[t_selb])
                sch.op("dve", lambda: V.scalar_tensor_tensor(out=gsel[:, :], in0=sc[:, :], scalar=1.0, in1=sel[:, :],
                                                             op0=ALU.mult, op1=ALU.mult, accum_out=sm[:, 48:49]),
                       reads=[t_sc, t_sel], writes=[t_gsel, t_sm])
                sch.op("dve", lambda: V.reciprocal(out=sm[:, 49:50], in_=sm[:, 48:49]), reads=[t_sm], writes=[t_sm])
                sch.op("dve", lambda: V.tensor_scalar(out=gsel[:, :], in0=gsel[:, :], scalar1=sm[:, 49:50], scalar2=2.5,
                                                      op0=ALU.mult, op1=ALU.mult), reads=[t_gsel, t_sm], writes=[t_gsel])
                sch.op("dve", lambda: V.tensor_tensor(out=sval[:, :], in0=sel[:, :], in1=k.eplus1, op=ALU.mult),
                       reads=[t_sel, k.t_cg], writes=[t_sval])
                sch.op("dve", lambda: V.max(out=sm[:, 40:48], in_=sval[:, :]), reads=[t_sval], writes=[t_sm])
                for j in range(8):
                    sch.op("dve", lambda gt=gt, j=j: V.scalar_tensor_tensor(
                        out=junk[:, :], in0=sval[:, :], scalar=sm[:, 40 + j:41 + j], in1=gsel[:, :],
                        op0=ALU.is_equal, op1=ALU.mult, accum_out=k.gate_all[:, gt, j:j + 1]),
                        reads=[t_sval, t_sm, t_gsel], writes=[t_junk, k.t_gate])
                def count_ops():
                    br2 = nbank()
                    sch.op("pe", lambda br2=br2: nc.tensor.matmul(banks[br2][:, 0:NE], lhsT=k.cb[:, 896:1024], rhs=selb[:, :],
                                                                   start=True, stop=True),
                           reads=[t_selb, k.t_cb], writes=[t_banks[br2]])
                    sch.op("dve", lambda br2=br2: nc.vector.tensor_tensor(out=k.base[:, :], in0=banks[br2][:, 0:NE], in1=k.base[:, :],
                                                                          op=ALU.add),
                           reads=[t_banks[br2], k.t_base], writes=[k.t_base])
                pending.append(count_ops)
                sch.op("sp", lambda gt=gt: nc.sync.dma_start(out=k.SELB[gt, :, :], in_=selb[:, :]), reads=[t_selb], dma=True)
                sch.op("sp", lambda gt=gt, si=si: nc.sync.dma_start(out=k.H2TOK[gt * 128:(gt + 1) * 128, :], in_=h2tok[si][:, :]),
                       reads=[t_h2tok[si]], dma=True)
        while pending:
            pending.pop(0)()
        sch.flush("F1")


def phase_F2(k):
    nc, sch = k.nc, k.sch
    with ExitStack() as st:
        sb, ps = k.sb, k.ps
        ntl = sb(st, "ntl", [128, NE], F32)
        ca = sb(st, "ca", [128, NE], F32)
        cbb = sb(st, "cbb", [128, NE], F32)
        tecol = sb(st, "tecol", [128, 2], F32)
        ind = [sb(st, f"ind{i}", [128, NBLK], BF16) for i in range(2)]
        idxf = sb(st, "idxf", [128, NBLK], F32)
        jk = sb(st, "jk2", [128, 128], F32)
        pb = ps(st, "pbexp", [128, NBLK])
        T = Tok
        t_ntl, t_ca, t_cbb, t_tecol, t_idxf, t_jk, t_pb = (T() for _ in range(7))
        t_ind = [T(), T()]
        V = nc.vector
        sch.op("dve", lambda: V.tensor_scalar(out=ntl[:, :], in0=k.base[:, :], scalar1=0.0, scalar2=None, op0=ALU.is_gt),
               reads=[k.t_base], writes=[t_ntl])
        for m in range(1, S // BS):
            sch.op("dve", lambda m=m: V.scalar_tensor_tensor(out=ntl[:, :], in0=k.base[:, :], scalar=float(BS) * m, in1=ntl[:, :],
                                                             op0=ALU.is_gt, op1=ALU.add),
                   reads=[k.t_base, t_ntl], writes=[t_ntl])
        src, tsrc = ntl, t_ntl
        bufs = [(ca, t_ca), (cbb, t_cbb)]
        for si, sh in enumerate((1, 2, 4, 8, 16, 32, 64, 128)):
            dst, tdst = bufs[si % 2]
            sch.op("dve", lambda src=src, dst=dst, sh=sh: V.tensor_tensor(
                out=dst[:, sh:NE], in0=src[:, sh:NE], in1=src[:, 0:NE - sh], op=ALU.add),
                reads=[tsrc], writes=[tdst])
            sch.op("dve", lambda src=src, dst=dst, sh=sh: V.tensor_copy(out=dst[:, 0:sh], in_=src[:, 0:sh]),
                   reads=[tsrc], writes=[tdst])
            src, tsrc = dst, tdst
        tend, t_tend = src, tsrc
        sch.op("dve", lambda: V.tensor_tensor(out=k.base[:, :], in0=tend[:, :], in1=ntl[:, :], op=ALU.subtract),
               reads=[t_tend, t_ntl], writes=[k.t_base])
        sch.op("dve", lambda: V.tensor_scalar(out=k.base[:, :], in0=k.base[:, :], scalar1=float(BS), scalar2=1.0,
                                              op0=ALU.mult, op1=ALU.add), reads=[k.t_base], writes=[k.t_base])
        for q in range(2):
            sch.op("dve", lambda q=q: V.scalar_tensor_tensor(
                out=jk[:, :], in0=tend[:, q * 128:(q + 1) * 128], scalar=1.0, in1=k.ident_f, op0=ALU.mult, op1=ALU.mult,
                accum_out=tecol[:, q:q + 1]), reads=[t_tend, k.t_cf], writes=[t_jk, t_tecol])
        for q in range(2):
            sch.op("dve", lambda q=q: V.tensor_scalar(out=ind[q][:, :], in0=k.jrow[:, 0:NBLK], scalar1=tecol[:, q:q + 1], scalar2=None,
                                                      op0=ALU.is_ge), reads=[t_tecol, k.t_cg], writes=[t_ind[q]])
            sch.op("pe", lambda q=q: nc.tensor.matmul(pb[:, :], lhsT=k.cb[:, 896:1024], rhs=ind[q][:, :],
                                                       start=(q == 0), stop=(q == 1)),
                   reads=[t_ind[q], k.t_cb], writes=[t_pb])
        sch.op("dve", lambda: V.tensor_scalar(out=idxf[:, :], in0=pb[:, :], scalar1=128.0, scalar2=None,
                                              op0=ALU.mult), reads=[t_pb], writes=[t_idxf])
        sch.op("dve", lambda: V.tensor_scalar(out=idxf[:, :], in0=idxf[:, :], scalar1=k.pcol, scalar2=None,
                                              op0=ALU.add), reads=[t_idxf, k.t_cg], writes=[t_idxf])
        sch.op("dve", lambda: V.tensor_copy(out=k.idx_all[:, :], in_=idxf[:, :]), reads=[t_idxf], writes=[k.t_idx])
        if "IDX" in k.debug:
            sch.op("sp", lambda: nc.sync.dma_start(out=k.IDX[:, :], in_=k.idx_all[:, :]), reads=[k.t_idx], dma=True)
        sch.flush("F2")


def phase_F3(k):
    nc, sch = k.nc, k.sch
    with ExitStack() as st:
        sb, ps = k.sb, k.ps
        selb = [sb(st, f"selb3{i}", [128, NE], BF16) for i in range(2)]
        h2t = [sb(st, f"h2t3{i}", [128, D], BF16) for i in range(3)]
        sval = [sb(st, f"sval3{i}", [128, NE], F32) for i in range(2)]
        s8 = [sb(st, f"s83{i}", [128, 8], F32) for i in range(2)]
        pr1 = [ps(st, f"pr1{i}", [128, NE]) for i in range(2)]
        pr2 = [ps(st, f"pr2{i}", [128, NE]) for i in range(2)]
        T = Tok
        t_selb, t_sval, t_s8, t_pr1, t_pr2 = ([T(), T()] for _ in range(5))
        t_h2t = [T(), T(), T()]
        V = nc.vector
        for gt in range(NT):
            b = gt % 2
            hb = gt % 3
            sch.op("sp", lambda gt=gt, b=b: nc.sync.dma_start(out=selb[b][:, :], in_=k.SELB[gt, :, :]), writes=[t_selb[b]], dma=True)
            sch.op("sp", lambda gt=gt, hb=hb: nc.sync.dma_start(out=h2t[hb][:, :], in_=k.H2TOK[gt * 128:(gt + 1) * 128, :]),
                   writes=[t_h2t[hb]], dma=True)
            sch.op("pe", lambda b=b: nc.tensor.matmul(pr1[b][:, :], lhsT=k.Ustrict, rhs=selb[b][:, :], start=True, stop=True),
                   reads=[t_selb[b], k.t_cb], writes=[t_pr1[b]])
            sch.op("pe", lambda b=b: nc.tensor.matmul(pr2[b][:, :], lhsT=k.cb[:, 896:1024], rhs=selb[b][:, :], start=True, stop=True),
                   reads=[t_selb[b], k.t_cb], writes=[t_pr2[b]])
            sch.op("dve", lambda b=b: V.tensor_tensor(out=sval[b][:, :], in0=pr1[b][:, :], in1=k.base[:, :], op=ALU.add),
                   reads=[t_pr1[b], k.t_base], writes=[t_sval[b]])
            sch.op("dve", lambda b=b: V.tensor_tensor(out=sval[b][:, :], in0=sval[b][:, :], in1=selb[b][:, :], op=ALU.mult),
                   reads=[t_sval[b], t_selb[b]], writes=[t_sval[b]])
            sch.op("dve", lambda b=b: V.tensor_tensor(out=k.base[:, :], in0=pr2[b][:, :], in1=k.base[:, :], op=ALU.add),
                   reads=[t_pr2[b], k.t_base], writes=[k.t_base])
            sch.op("dve", lambda b=b: V.max(out=s8[b][:, :], in_=sval[b][:, :]), reads=[t_sval[b]], writes=[t_s8[b]])
            sch.op("dve", lambda gt=gt, b=b: V.tensor_scalar(out=k.slot_all[:, gt, :], in0=s8[b][:, :], scalar1=-1.0, scalar2=None,
                                                             op0=ALU.add), reads=[t_s8[b]], writes=[k.t_slot])
            for j in range(8):
                sch.op("pool", lambda gt=gt, j=j, hb=hb: nc.gpsimd.indirect_dma_start(
                    out=k.XG[:, :], out_offset=bass.IndirectOffsetOnAxis(ap=k.slot_all[:, gt, j:j + 1], axis=0),
                    in_=h2t[hb][:, :], in_offset=None),
                    reads=[k.t_slot, t_h2t[hb]], dma=True)
        if "SLOT" in k.debug:
            sch.op("sp", lambda: nc.sync.dma_start(out=k.SLOT[:, :, :], in_=k.slot_all[:, :, :]), reads=[k.t_slot], dma=True)
            sch.op("sp", lambda: nc.sync.dma_start(out=k.GATE[:, :, :], in_=k.gate_all[:, :, :]), reads=[k.t_gate], dma=True)
        sch.flush("F3")


def phase_G(k):
    nc, sch = k.nc, k.sch
    with ExitStack() as st:
        sb, ps = k.sb, k.ps
        wf = [sb(st, f"wfa{i}", [128, 6144], F32) for i in range(4)]
        wgb = [sb(st, f"wgb{i}", [128, 8, 256], BF16) for i in range(2)]
        wub = [sb(st, f"wub{i}", [128, 8, 256], BF16) for i in range(2)]
        wdb = [sb(st, f"wdb{i}", [128, 2, D], BF16) for i in range(2)]
        xg = [sb(st, f"xg{i}", [128, TPB, D], BF16) for i in range(4)]
        xgT = [sb(st, f"xgT{i}", [128, 8, BS], BF16) for i in range(2)]
        sgt = [sb(st, f"sgt{i}", [128, 2, BS], F32) for i in range(2)]
        hidT = [sb(st, f"hidT{i}", [128, 2, BS], BF16) for i in range(2)]
        ysb = [sb(st, f"ysb{i}", [128, D], BF16) for i in range(3)]
        pT = [ps(st, f"gpT{i}", [128, D], BF16) for i in range(2)]
        pG = [ps(st, f"gpG{i}", [128, 2, BS]) for i in range(2)]
        pU = [ps(st, f"gpU{i}", [128, 2, BS]) for i in range(2)]
        pY = [ps(st, f"gpY{i}", [128, 512]) for i in range(2)]
        T = Tok
        t_wf = [T(), T(), T(), T()]
        t_wgb, t_wub, t_wdb = ([T(), T()] for _ in range(3))
        t_xg = [T() for _ in range(4)]
        t_xgT, t_sgt, t_hidT = [T(), T()], [T(), T()], [T(), T()]
        t_ysb = [T() for _ in range(3)]
        t_pT = [T(), T()]
        t_pG, t_pU = [T(), T()], [T(), T()]
        t_pY = [T() for _ in range(2)]
        cnt = {"py": 0}
        NROW = NE * 128
        regs = {}

        def gather(j, q):
            f = q % 4
            off = bass.IndirectOffsetOnAxis(ap=k.idx_all[:, j:j + 1], axis=0)

            def fn(f=f, off=off):
                if "r" not in regs:
                    regs["r"] = nc.gpsimd.to_reg(NROW - 1)
                return nc.gpsimd.indirect_dma_start(out=wf[f][:, :], out_offset=None, in_=k.w_all[:, :], in_offset=off,
                                                    bounds_check=regs["r"], oob_is_err=False)
            sch.op("pool", fn, reads=[k.t_idx], writes=[t_wf[f]], dma=True)

        def xload(j, q):
            xi = q % 4
            sch.op("sp", lambda xi=xi, row0=j * BS: nc.sync.dma_start(
                out=xg[xi][:, :, :], in_=k.XG[row0:row0 + BS, :].rearrange("(t p) d -> p t d", p=128)),
                writes=[t_xg[xi]], dma=True)

        def cast(j, which):
            f, b = j % 4, j % 2
            if which == "g":
                sch.op("dve", lambda: nc.vector.tensor_copy(
                    out=wgb[b][:, :, :], in_=wf[f][:, 0:2048].rearrange("p (a c) -> p a c", a=8)),
                    reads=[t_wf[f]], writes=[t_wgb[b]])
            elif which == "u":
                sch.op("act", lambda: nc.scalar.copy(
                    out=wub[b][:, :, :], in_=wf[f][:, 2048:4096].rearrange("p (a c) -> p a c", a=8)),
                    reads=[t_wf[f]], writes=[t_wub[b]])
            elif which == "d0":
                sch.op("dve", lambda: nc.vector.tensor_copy(out=wdb[b][:, 0, :], in_=wf[f][:, 4096:5120]),
                       reads=[t_wf[f]], writes=[t_wdb[b]])
            else:
                sch.op("act", lambda: nc.scalar.copy(out=wdb[b][:, 1, :], in_=wf[f][:, 5120:6144]),
                       reads=[t_wf[f]], writes=[t_wdb[b]])

        tcnt = {"n": 0, "y": 0}

        def t_stage(j):
            xi, b = j % 4, j % 2
            for t in range(TPB):
                pi = tcnt["n"] % 2
                tcnt["n"] += 1
                for kk in range(8):
                    sch.op("pe", lambda kk=kk, t=t, pi=pi: nc.tensor.transpose(
                        out=pT[pi][:, kk * 128:(kk + 1) * 128], in_=xg[xi][:, t, kk * 128:(kk + 1) * 128], identity=k.ident_b),
                        reads=[t_xg[xi], k.t_cb], writes=[t_pT[pi]])
                srcv = pT[pi][:, :].rearrange("p (kk s) -> p kk s", kk=8)
                dstv = xgT[b][:, :, t * 128:(t + 1) * 128]
                if pi == 0:
                    sch.op("act", lambda srcv=srcv, dstv=dstv: nc.scalar.copy(out=dstv, in_=srcv), reads=[t_pT[pi]], writes=[t_xgT[b]])
                else:
                    sch.op("dve", lambda srcv=srcv, dstv=dstv: nc.vector.tensor_copy(out=dstv, in_=srcv), reads=[t_pT[pi]], writes=[t_xgT[b]])

        def gu_stage(j):
            b = j % 2
            for (pp, tp, wb_, twb) in ((pG[b], t_pG[b], wgb, t_wgb), (pU[b], t_pU[b], wub, t_wub)):
                for jc in range(2):
                    for kk in range(8):
                        sch.op("pe", lambda pp=pp, wb_=wb_, jc=jc, kk=kk: nc.tensor.matmul(
                            pp[:, jc, :], lhsT=wb_[b][:, kk, jc * 128:(jc + 1) * 128], rhs=xgT[b][:, kk, :],
                            start=(kk == 0), stop=(kk == 7)),
                            reads=[twb[b], t_xgT[b]], writes=[tp])
            sch.op("act", lambda: nc.scalar.activation(out=sgt[b][:, :, :], in_=pG[b][:, :, :], func=AF.Silu),
                   reads=[t_pG[b]], writes=[t_sgt[b]])
            sch.op("dve", lambda: nc.vector.tensor_tensor(
                out=hidT[b][:, :, :], in0=pU[b][:, :, :], in1=sgt[b][:, :, :], op=ALU.mult),
                reads=[t_pU[b], t_sgt[b]], writes=[t_hidT[b]])

        def d_stage(q, j):
            b = q % 2
            for t in range(TPB):
                yi = tcnt["y"] % 3
                tcnt["y"] += 1
                for hf in range(2):
                    for jc in range(2):
                        sch.op("pe", lambda jc=jc, hf=hf, t=t: nc.tensor.matmul(
                            pY[hf][:, :], lhsT=hidT[b][:, jc, t * 128:(t + 1) * 128], rhs=wdb[b][:, jc, hf * 512:(hf + 1) * 512],
                            start=(jc == 0), stop=(jc == 1)),
                            reads=[t_hidT[b], t_wdb[b]], writes=[t_pY[hf]])
                    if hf == 0:
                        sch.op("act", lambda yi=yi: nc.scalar.copy(out=ysb[yi][:, 0:512], in_=pY[0][:, :]),
                               reads=[t_pY[0]], writes=[t_ysb[yi]])
                    else:
                        sch.op("dve", lambda yi=yi: nc.vector.tensor_copy(out=ysb[yi][:, 512:1024], in_=pY[1][:, :]),
                               reads=[t_pY[1]], writes=[t_ysb[yi]])
                sch.op("sp", lambda yi=yi, row0=j * BS + t * 128: nc.sync.dma_start(out=k.YE[row0:row0 + 128, :], in_=ysb[yi][:, :]),
                       reads=[t_ysb[yi]], dma=True)

        order = []
        lo, hi = 0, NBLK - 1
        while lo <= hi:
            order.append(lo)
            lo += 1
            if lo <= hi:
                order.append(hi)
                hi -= 1
        def at(q):
            return order[q]

        NQ = len(order)
        for q0 in range(3):
            gather(at(q0), q0)
            xload(at(q0), q0)
        for w_ in ("g", "u", "d0", "d1"):
            cast(0, w_)
        cast(1, "g")
        cast(1, "u")
        t_stage(0)
        t_stage(1)
        gu_stage(0)
        cast(1, "d0")
        cast(1, "d1")
        for q in range(NQ):
            if q + 3 < NQ:
                gather(at(q + 3), q + 3)
                xload(at(q + 3), q + 3)
            if q + 2 < NQ:
                t_stage(q + 2)
            if q + 1 < NQ:
                gu_stage(q + 1)
            if q + 2 < NQ:
                cast(q + 2, "g")
                cast(q + 2, "u")
            d_stage(q, at(q))
            if q + 2 < NQ:
                cast(q + 2, "d0")
                cast(q + 2, "d1")
        sch.flush("G")


def phase_H(k):
    nc, sch = k.nc, k.sch
    with ExitStack() as st:
        sb = k.sb
        nf = sb(st, "nf", [128, D], F32)
        xs_t = [sb(st, f"hxs{i}", [128, D], F32) for i in range(2)]
        yk = [sb(st, f"yk{i}", [128, D], BF16) for i in range(16)]
        acc = [sb(st, f"acc{i}", [128, D], F32) for i in range(2)]
        sqj = sb(st, "sqj", [128, D], F32)
        ot = [sb(st, f"ot{i}", [128, D], F32) for i in range(2)]
        stat = sb(st, "stat", [128, 4], F32)
        T = Tok
        t_nf, t_sqj, t_stat = T(), T(), T()
        t_xs, t_acc, t_ot = [T(), T()], [T(), T()], [T(), T()]
        t_yk = [T() for _ in range(16)]
        sch.op("sp", lambda: nc.sync.dma_start(out=nf[:, :], in_=k.nfin_bc[:, :]), writes=[t_nf], dma=True)
        V = nc.vector
        for gt in range(NT):
            b = gt % 2
            sch.op("sp", lambda gt=gt, b=b: nc.sync.dma_start(out=xs_t[b][:, :], in_=k.XS[gt * 128:(gt + 1) * 128, :]),
                   writes=[t_xs[b]], dma=True)
            for j in range(8):
                yi = b * 8 + j
                sch.op("pool", lambda gt=gt, j=j, yi=yi: nc.gpsimd.indirect_dma_start(
                    out=yk[yi][:, :], out_offset=None, in_=k.YE[:, :],
                    in_offset=bass.IndirectOffsetOnAxis(ap=k.slot_all[:, gt, j:j + 1], axis=0)),
                    reads=[k.t_slot], writes=[t_yk[yi]], dma=True)
            for j in range(8):
                yi = b * 8 + j
                if j == 0:
                    sch.op("dve", lambda gt=gt, b=b, yi=yi: V.tensor_scalar(
                        out=acc[b][:, :], in0=yk[yi][:, :], scalar1=k.gate_all[:, gt, 0:1], scalar2=None, op0=ALU.mult),
                        reads=[t_yk[yi], k.t_gate], writes=[t_acc[b]])
                else:
                    sch.op("dve", lambda gt=gt, b=b, yi=yi, j=j: V.scalar_tensor_tensor(
                        out=acc[b][:, :], in0=yk[yi][:, :], scalar=k.gate_all[:, gt, j:j + 1], in1=acc[b][:, :],
                        op0=ALU.mult, op1=ALU.add),
                        reads=[t_yk[yi], k.t_gate, t_acc[b]], writes=[t_acc[b]])
            sch.op("dve", lambda b=b: V.tensor_tensor(out=acc[b][:, :], in0=acc[b][:, :], in1=k.gfbc[:, :], op=ALU.mult),
                   reads=[t_acc[b], k.t_gfbc], writes=[t_acc[b]])
            sch.op("dve", lambda b=b: V.tensor_tensor(out=acc[b][:, :], in0=acc[b][:, :], in1=xs_t[b][:, :], op=ALU.add),
                   reads=[t_acc[b], t_xs[b]], writes=[t_acc[b]])
            sch.op("act", lambda b=b: nc.scalar.activation(out=sqj[:, :], in_=acc[b][:, :], func=AF.Square, accum_out=stat[:, 0:1]),
                   reads=[t_acc[b]], writes=[t_sqj, t_stat])
            sch.op("act", lambda: nc.scalar.activation(out=stat[:, 1:2], in_=stat[:, 0:1], func=AF.Sqrt, scale=1.0 / D, bias=EPS),
                   reads=[t_stat], writes=[t_stat])
            sch.op("dve", lambda: V.reciprocal(out=stat[:, 2:3], in_=stat[:, 1:2]), reads=[t_stat], writes=[t_stat])
            sch.op("dve", lambda b=b: V.scalar_tensor_tensor(
                out=ot[b][:, :], in0=acc[b][:, :], scalar=stat[:, 2:3], in1=nf[:, :], op0=ALU.mult, op1=ALU.mult),
                reads=[t_acc[b], t_stat, t_nf], writes=[t_ot[b]])
            sch.op("sp", lambda gt=gt, b=b: nc.sync.dma_start(out=k.out[gt * 128:(gt + 1) * 128, :], in_=ot[b][:, :]),
                   reads=[t_ot[b]], dma=True)
        sch.flush("H")
```
